# Optimizing a Trainium2 kernel written in Bass

```python
import math
import jax, jax.numpy as jnp
from jax import lax
import numpy as np

D_MODEL = 1024
BATCH = 2
SEQ = 8192
DEPTH = 1

N_HEADS = 8
HEAD_DIM = 64
ATT_WIDTH = N_HEADS * HEAD_DIM
LRU_WIDTH = 1024
LRU_BLOCKS = 8
LRU_BLOCK_DIM = LRU_WIDTH // LRU_BLOCKS
LRU_C = 8.0
CONV_WIDTH = 4
IN_PROJ = 3 * ATT_WIDTH + 2 * LRU_WIDTH
PL_DIM = 256
N_EXPERTS = 32
TOP_K = 4
D_EXPERT = 1024
SWIGLU_LIMIT = 7.0
SWIGLU_ALPHA = 1.702
Q_BLOCK = 128
MOE_BLOCK = 128
EPS = 1e-6

kernel_name = "hybrid_stickbreak_rglru_moe_block"


def rmsnorm(x, g):
    x32 = x.astype(jnp.float32)
    y = x32 * lax.rsqrt(jnp.mean(x32 * x32, axis=-1, keepdims=True) + EPS) * g.astype(jnp.float32)
    return y.astype(x.dtype)


def stick_breaking_attention(q, k, v):
    B, S, _ = q.shape
    n_qb = S // Q_BLOCK
    qh = q.reshape(B, S, N_HEADS, HEAD_DIM).astype(jnp.float32) * (1.0 / math.sqrt(HEAD_DIM))
    kh = k.reshape(B, S, N_HEADS, HEAD_DIM).astype(jnp.float32)
    vh = v.reshape(B, S, N_HEADS, HEAD_DIM).astype(jnp.float32)
    qb = qh.reshape(B, n_qb, Q_BLOCK, N_HEADS, HEAD_DIM).transpose(1, 0, 3, 2, 4)
    kpos = jnp.arange(S)

    def block(args):
        q_blk, blk = args
        qpos = blk * Q_BLOCK + jnp.arange(Q_BLOCK)
        z = jnp.einsum('bhqd,bkhd->bhqk', q_blk, kh)
        mask = kpos[None, :] < qpos[:, None]
        log_keep = jnp.where(mask, jax.nn.log_sigmoid(-z), 0.0)
        log_between = lax.cumsum(log_keep, axis=3, reverse=True) - log_keep
        w = jnp.where(mask, jnp.exp(jax.nn.log_sigmoid(z) + log_between), 0.0)
        return jnp.einsum('bhqk,bkhd->bqhd', w, vh)

    out = lax.map(block, (qb, jnp.arange(n_qb)))
    return out.transpose(1, 0, 2, 3, 4).reshape(B, S, ATT_WIDTH).astype(q.dtype)


def causal_depthwise_conv(u, w, b):
    S = u.shape[1]
    up = jnp.pad(u, ((0, 0), (CONV_WIDTH - 1, 0), (0, 0)))
    out = b
    for j in range(CONV_WIDTH):
        out = out + up[:, j:j + S] * w[j]
    return out


def rglru(u, wa, ba, wx, bx, lam):
    B, S, W = u.shape
    ub = u.reshape(B, S, LRU_BLOCKS, LRU_BLOCK_DIM)
    r = jax.nn.sigmoid(jnp.einsum('bsnc,ncd->bsnd', ub, wa).reshape(B, S, W) + ba)
    i = jax.nn.sigmoid(jnp.einsum('bsnc,ncd->bsnd', ub, wx).reshape(B, S, W) + bx)
    log_a = LRU_C * r.astype(jnp.float32) * jax.nn.log_sigmoid(lam.astype(jnp.float32))
    a = jnp.exp(log_a)
    b_in = jnp.sqrt(-jnp.expm1(2.0 * log_a)) * (i * u).astype(jnp.float32)

    def step(h, ab):
        a_t, b_t = ab
        h = a_t * h + b_t
        return h, h

    _, hs = lax.scan(step, jnp.zeros((B, W), jnp.float32),
                     (a.transpose(1, 0, 2), b_in.transpose(1, 0, 2)))
    return hs.transpose(1, 0, 2).astype(u.dtype)


def token_mixing(xn, w_in, conv_w, conv_b, lru_wa, lru_ba, lru_wx, lru_bx, lru_lambda,
                 w_att_out, w_lru_out, w_merge, b_merge, w_out):
    proj = xn @ w_in
    q, k, v, u, g_in = jnp.split(
        proj, [ATT_WIDTH, 2 * ATT_WIDTH, 3 * ATT_WIDTH, 3 * ATT_WIDTH + LRU_WIDTH], axis=-1)
    y_att = stick_breaking_attention(q, k, v) @ w_att_out
    h_lru = rglru(causal_depthwise_conv(u, conv_w, conv_b), lru_wa, lru_ba, lru_wx, lru_bx, lru_lambda)
    y_lru = (jax.nn.gelu(g_in) * h_lru) @ w_lru_out
    gates = jax.nn.sigmoid(xn @ w_merge + b_merge)
    g_att, g_lru = jnp.split(gates, 2, axis=-1)
    return (g_att * y_att + g_lru * y_lru) @ w_out


def clamped_swiglu_expert(xb, w_gu, b_gu, w_dn, b_dn):
    hgu = xb @ w_gu + b_gu
    gate, up = jnp.split(hgu, 2, axis=-1)
    gate = jnp.minimum(gate, SWIGLU_LIMIT)
    up = jnp.clip(up, -SWIGLU_LIMIT, SWIGLU_LIMIT)
    glu = gate * jax.nn.sigmoid(gate * SWIGLU_ALPHA)
    return ((up + 1.0) * glu) @ w_dn + b_dn


def moe(xn, w_router, b_router, w_gate_up, b_gate_up, w_down, b_down):
    B, S, D = xn.shape
    n_tok = B * S
    xf = xn.reshape(n_tok, D)
    logits = (xf @ w_router + b_router).astype(jnp.float32)
    top_v, top_e = lax.top_k(logits, TOP_K)
    gates = jax.nn.softmax(top_v, axis=-1)
    n_asg = n_tok * TOP_K
    e_flat = top_e.reshape(n_asg)
    tok_flat = jnp.arange(n_asg, dtype=jnp.int32) // TOP_K
    g_flat = gates.reshape(n_asg)
    order = jnp.argsort(e_flat)
    e_sorted, tok_sorted, g_sorted = e_flat[order], tok_flat[order], g_flat[order]
    counts = jnp.bincount(e_flat, length=N_EXPERTS)
    padded = (counts + MOE_BLOCK - 1) // MOE_BLOCK * MOE_BLOCK
    start = jnp.cumsum(counts) - counts
    pad_end = jnp.cumsum(padded)
    pad_start = pad_end - padded
    dest = pad_start[e_sorted] + jnp.arange(n_asg) - start[e_sorted]
    n_rows = (n_asg + MOE_BLOCK - 1) // MOE_BLOCK * MOE_BLOCK + N_EXPERTS * MOE_BLOCK
    n_blocks = n_rows // MOE_BLOCK
    row_tok = jnp.zeros((n_rows,), jnp.int32).at[dest].set(tok_sorted)
    row_gate = jnp.zeros((n_rows,), jnp.float32).at[dest].set(g_sorted)
    block_expert = jnp.minimum(
        jnp.searchsorted(pad_end, jnp.arange(n_blocks) * MOE_BLOCK, side='right'), N_EXPERTS - 1)
    xb = xf[row_tok].reshape(n_blocks, MOE_BLOCK, D)

    def expert_block(args):
        x_blk, e = args
        return clamped_swiglu_expert(x_blk, w_gate_up[e], b_gate_up[e], w_down[e], b_down[e])

    yb = lax.map(expert_block, (xb, block_expert))
    y_rows = yb.reshape(n_rows, D).astype(jnp.float32) * row_gate[:, None]
    y = jax.ops.segment_sum(y_rows, row_tok, num_segments=n_tok)
    return y.reshape(B, S, D).astype(xn.dtype)


def setup_inputs(seed: int = 0) -> dict:
    key = jax.random.key(seed)
    ks = jax.random.split(key, 32)

    def nrm(k, shape, scale):
        return jax.random.normal(k, shape, jnp.float32) * scale

    u = jax.random.uniform(ks[10], (DEPTH, LRU_WIDTH), jnp.float32, minval=0.9, maxval=0.999)
    a0 = u ** (1.0 / LRU_C)
    lru_lambda = jnp.log(a0) - jnp.log1p(-a0)
    return {
        "x": nrm(ks[0], (BATCH, SEQ, D_MODEL), 1.0),
        "p": nrm(ks[1], (DEPTH, BATCH, SEQ, PL_DIM), 1.0),
        "norm_mix_g": 1.0 + nrm(ks[2], (DEPTH, D_MODEL), 0.02),
        "w_in": nrm(ks[3], (DEPTH, D_MODEL, IN_PROJ), D_MODEL ** -0.5),
        "conv_w": nrm(ks[4], (DEPTH, CONV_WIDTH, LRU_WIDTH), CONV_WIDTH ** -0.5),
        "conv_b": nrm(ks[5], (DEPTH, LRU_WIDTH), 0.01),
        "lru_wa": nrm(ks[6], (DEPTH, LRU_BLOCKS, LRU_BLOCK_DIM, LRU_BLOCK_DIM), LRU_BLOCK_DIM ** -0.5),
        "lru_ba": nrm(ks[7], (DEPTH, LRU_WIDTH), 0.01),
        "lru_wx": nrm(ks[8], (DEPTH, LRU_BLOCKS, LRU_BLOCK_DIM, LRU_BLOCK_DIM), LRU_BLOCK_DIM ** -0.5),
        "lru_bx": nrm(ks[9], (DEPTH, LRU_WIDTH), 0.01),
        "lru_lambda": lru_lambda,
        "w_att_out": nrm(ks[11], (DEPTH, ATT_WIDTH, D_MODEL), ATT_WIDTH ** -0.5),
        "w_lru_out": nrm(ks[12], (DEPTH, LRU_WIDTH, D_MODEL), LRU_WIDTH ** -0.5),
        "w_merge": nrm(ks[13], (DEPTH, D_MODEL, 2 * D_MODEL), D_MODEL ** -0.5),
        "b_merge": nrm(ks[14], (DEPTH, 2 * D_MODEL), 0.01),
        "w_out": nrm(ks[15], (DEPTH, D_MODEL, D_MODEL), D_MODEL ** -0.5),
        "norm_moe_g": 1.0 + nrm(ks[16], (DEPTH, D_MODEL), 0.02),
        "w_router": nrm(ks[17], (DEPTH, D_MODEL, N_EXPERTS), D_MODEL ** -0.5),
        "b_router": nrm(ks[18], (DEPTH, N_EXPERTS), 0.01),
        "w_gate_up": nrm(ks[19], (DEPTH, N_EXPERTS, D_MODEL, 2 * D_EXPERT), D_MODEL ** -0.5),
        "b_gate_up": nrm(ks[20], (DEPTH, N_EXPERTS, 2 * D_EXPERT), 0.01),
        "w_down": nrm(ks[21], (DEPTH, N_EXPERTS, D_EXPERT, D_MODEL), D_EXPERT ** -0.5),
        "b_down": nrm(ks[22], (DEPTH, N_EXPERTS, D_MODEL), 0.01),
        "norm_pl_g": 1.0 + nrm(ks[23], (DEPTH, D_MODEL), 0.02),
        "w_pl_gate": nrm(ks[24], (DEPTH, D_MODEL, D_MODEL), D_MODEL ** -0.5),
        "b_pl_gate": nrm(ks[25], (DEPTH, D_MODEL), 0.01),
        "w_pl_proj": nrm(ks[26], (DEPTH, PL_DIM, D_MODEL), PL_DIM ** -0.5),
        "norm_pl_post_g": 1.0 + nrm(ks[27], (DEPTH, D_MODEL), 0.02),
        "norm_final_g": 1.0 + nrm(ks[28], (D_MODEL,), 0.02),
    }


def reference(x, p, norm_mix_g, w_in, conv_w, conv_b, lru_wa, lru_ba, lru_wx, lru_bx, lru_lambda,
              w_att_out, w_lru_out, w_merge, b_merge, w_out, norm_moe_g, w_router, b_router,
              w_gate_up, b_gate_up, w_down, b_down, norm_pl_g, w_pl_gate, b_pl_gate, w_pl_proj,
              norm_pl_post_g, norm_final_g):
    h = x
    for i in range(DEPTH):
        xn = rmsnorm(h, norm_mix_g[i])
        h = h + token_mixing(xn, w_in[i], conv_w[i], conv_b[i], lru_wa[i], lru_ba[i], lru_wx[i],
                             lru_bx[i], lru_lambda[i], w_att_out[i], w_lru_out[i], w_merge[i],
                             b_merge[i], w_out[i])
        xn = rmsnorm(h, norm_moe_g[i])
        h = h + moe(xn, w_router[i], b_router[i], w_gate_up[i], b_gate_up[i], w_down[i], b_down[i])
        hn = rmsnorm(h, norm_pl_g[i])
        pl_gate = jax.nn.sigmoid(hn @ w_pl_gate[i] + b_pl_gate[i])
        pl = rmsnorm(p[i].astype(h.dtype) @ w_pl_proj[i], norm_pl_post_g[i])
        h = h + pl_gate * pl
    return rmsnorm(h, norm_final_g)
```

```python
import numpy as np
import concourse.bass as bass
import concourse.mybir as mybir
from concourse.bass_utils import run_bass_kernel_spmd
from contextlib import ExitStack
import os
STOP = int(os.environ.get('KSTOP', '99'))
DBG = os.environ.get('KDBG', '') != ''
LAST = {}

F32 = mybir.dt.float32
BF16 = mybir.dt.bfloat16
AF = mybir.ActivationFunctionType
ALU = mybir.AluOpType
AX = mybir.AxisListType

ENG = ['pe', 'act', 'dve', 'pool', 'sp']
EOBJ = {'pe': 'tensor', 'act': 'scalar', 'dve': 'vector', 'pool': 'gpsimd', 'sp': 'sync'}
NDS = 8
EPS = 1e-6


class Prog:
    def __init__(self, nc, es):
        self.nc = nc
        self.ops = {e: [] for e in ENG}
        self.cnt = {e: 0 for e in ENG}
        self.sem = {e: es.enter_context(nc.semaphore("s_" + e)) for e in ENG}
        self.seen = {e: {} for e in ENG}
        self.dq = ('sp', 'pool', 'act')
        self.dsem = {q: [es.enter_context(nc.semaphore("d_%s%d" % (q, i))) for i in range(NDS)] for q in self.dq}
        self.dcnt = {q: 0 for q in self.dq}
        self.last_w = {}
        self.readers = {}

    def need(self, eng, tok):
        kind, src, n = tok
        if kind == 'e':
            if src == eng and eng == 'pe':
                return
            key = ('e', src)
            val = n
            sem = self.sem[src]
        else:
            slot = n % NDS
            key = ('d', src, slot)
            val = 16 * (n // NDS + 1)
            sem = self.dsem[src][slot]
        if self.seen[eng].get(key, 0) >= val:
            return
        self.seen[eng][key] = val
        self.ops[eng].append(('wait', sem, val))

    def _deps(self, eng, r, w):
        toks = []
        for k in r:
            if k in self.last_w:
                toks.append(self.last_w[k])
        for k in w:
            if k in self.last_w:
                toks.append(self.last_w[k])
            toks.extend(self.readers.get(k, {}).values())
        for t in toks:
            self.need(eng, t)

    def _mark(self, tok, r, w):
        for k in r:
            d = self.readers.setdefault(k, {})
            if tok[0] == 'e':
                d[('e', tok[1])] = tok
            else:
                d[tok] = tok
        for k in w:
            self.last_w[k] = tok
            self.readers[k] = {}

    def op(self, eng, fn, r=(), w=()):
        self._deps(eng, r, w)
        n = self.cnt[eng] + 1
        self.cnt[eng] = n
        self.ops[eng].append(('op', fn))
        self._mark(('e', eng, n), r, w)

    def dma(self, q, out_ap, in_ap, r=(), w=()):
        self._deps(q, r, w)
        i = self.dcnt[q]
        self.dcnt[q] = i + 1
        if i >= NDS:
            self.need(q, ('d', q, i - NDS))
        self.ops[q].append(('dma', out_ap, in_ap, i % NDS))
        self._mark(('d', q, i), r, w)

    def barrier(self):
        for e in ENG:
            for q in self.dq:
                for i in range(max(0, self.dcnt[q] - NDS), self.dcnt[q]):
                    self.need(e, ('d', q, i))
            for f in ENG:
                if f != e and self.cnt[f] > 0:
                    self.need(e, ('e', f, self.cnt[f]))
        self.last_w = {}
        self.readers = {}

    def emit(self):
        nc = self.nc
        with nc.Block() as block:
            for e in ENG:
                items = self.ops[e]

                def body(eng, e=e, items=items):
                    for item in items:
                        if item[0] == 'wait':
                            eng.wait_ge(item[1], item[2])
                        elif item[0] == 'op':
                            item[1](eng).then_inc(self.sem[e], 1)
                        else:
                            _, o, i_, slot = item
                            eng.dma_start(out=o, in_=i_).then_inc(self.dsem[e][slot], 16)
                getattr(block, EOBJ[e])(body)
        self.ops = {e: [] for e in ENG}


V_GMIX, V_GMOE, V_GPL, V_CW, V_CB, V_BA, V_BX, V_LAM, V_BM, V_BGU = 0, 8, 16, 24, 56, 64, 72, 80, 88, 104
NV = 104 + 512


def build_program():
    nc = bass.Bass("TRN2", target_bir_lowering=False)

    def din(name, shape, dt=F32):
        return nc.dram_tensor(name, shape, dt, kind="ExternalInput").ap()

    def dscr(name, shape, dt):
        return nc.dram_tensor(name, shape, dt, kind=("ExternalOutput" if DBG else "Internal")).ap()

    xb = din("xb", [8192, 1024]); xo = din("xo", [2048, 1024]); po = din("po", [2048, 256])
    masks_d = din("masks", [128, 1024]); sel_d = din("sel", [128, 8])
    ident_d = din("ident", [128, 128]); ntri_d = din("ntri", [128, 128]); vecs_d = din("vecs", [128, NV])
    w_in = din("w_in", [1024, 3584]); lru_wa = din("lru_wa", [8, 128, 128]); lru_wx = din("lru_wx", [8, 128, 128])
    w_att_out = din("w_att_out", [512, 1024]); w_lru_out = din("w_lru_out", [1024, 1024])
    w_merge = din("w_merge", [1024, 2048]); w_out = din("w_out", [1024, 1024])
    w_router = din("w_router", [1024, 32]); b_router = din("b_router", [1, 32])
    w_gu = din("w_gate_up", [32, 1024, 2048]); w_dn = din("w_down", [32, 1024, 1024]); b_dn = din("b_down", [32, 1024])
    w_plg = din("w_pl_gate", [1024, 1024]); b_plg = din("b_pl_gate", [1, 1024]); w_plp = din("w_pl_proj", [256, 1024])
    g_post = din("g_post", [1, 1024]); g_fin = din("g_fin", [1, 1024])
    out_d = nc.dram_tensor("out", [2048, 1024], F32, kind="ExternalOutput").ap()

    kT_d = dscr("kT_d", [4, 128, 8192], BF16); v_d = dscr("v_d", [64, 128, 512], BF16)
    hown_d = dscr("hown_d", [128, 8, 2048], BF16); qT_d = dscr("qT_d", [4, 128, 2048], BF16)
    hg_d = dscr("hg_d", [128, 8, 2048], BF16); gm_d = dscr("gm_d", [128, 16, 2048], BF16)
    attT_d = dscr("attT_d", [128, 4, 2048], BF16); h_d = dscr("h_d", [2048, 1024], F32)

    with ExitStack() as es:
        P = Prog(nc, es)
        _uid = [0]
        _orig_sbuf = nc.sbuf_tensor

        def _sbuf(name, shape, dt):
            _uid[0] += 1
            return _orig_sbuf("%s_%d" % (name, _uid[0]), shape, dt)
        PB = [es.enter_context(nc.psum_tensor("pb%d" % i, [128, 512], F32)) for i in range(6)]
        PTs = [es.enter_context(nc.psum_tensor("ptb%d" % i, [128, 1024], BF16)) for i in range(2)]
        PT = PTs[0]

        def sbp(name, shape, dt=F32):
            return es.enter_context(_sbuf(name, shape, dt))
        vec = sbp("vec", [128, NV]); identf = sbp("identf", [128, 128]); identb = sbp("identb", [128, 128], BF16)
        cL = sbp("cL", [128, 8]); tmp8 = sbp("tmp8", [128, 8])
        P.dma('sp', vec[:], vecs_d[:, :], w=['vec'])
        P.dma('sp', identf[:], ident_d[:, :], w=['identf'])
        P.op('dve', lambda e: e.tensor_copy(out=identb[:], in_=identf[:]), r=['identf'], w=['identb'])
        P.op('act', lambda e: e.activation(out=tmp8[:], in_=vec[:, V_LAM:V_LAM + 8], func=AF.Exp, scale=-1.0), r=['vec'], w=['tmp8'])
        P.op('act', lambda e: e.activation(out=tmp8[:], in_=tmp8[:], func=AF.Ln, bias=1.0), r=['tmp8'], w=['tmp8'])
        P.op('dve', lambda e: e.tensor_scalar(out=cL[:], in0=tmp8[:], scalar1=-8.0, scalar2=None, op0=ALU.mult), r=['tmp8'], w=['cL'])
        P.emit()

        def front_end(S, src_rows, gcol, nblk, tag, next_rows=None, first=True):
            xt, xs, xnT, junk, ss, ms, sq, rstd = S['xt'], S['xs'], S['xnT'], S['junk'], S['ss'], S['ms'], S['sq'], S['rstd']
            if first:
                P.dma('sp', xt[:, 0:nblk, :], src_rows.rearrange("(j p) d -> p j d", p=128), w=['xt'])
            for j in range(nblk):
                P.op('act', lambda e, j=j: e.activation(out=junk[:], in_=xt[:, j, :], func=AF.Square, accum_out=ss[:, j:j + 1]),
                     r=['xt'], w=['junk', ('ss', j)])
            P.op('dve', lambda e: e.tensor_scalar(out=ms[:, 0:nblk], in0=ss[:, 0:nblk], scalar1=1.0 / 1024, scalar2=EPS, op0=ALU.mult, op1=ALU.add),
                 r=[('ss', j) for j in range(nblk)], w=['ms'])
            P.op('act', lambda e: e.activation(out=sq[:, 0:nblk], in_=ms[:, 0:nblk], func=AF.Sqrt), r=['ms'], w=['sq'])
            P.op('dve', lambda e: e.reciprocal(out=rstd[:, 0:nblk], in_=sq[:, 0:nblk]), r=['sq'], w=['rstd'])
            for j in range(nblk):
                P.op('act', lambda e, j=j: e.activation(out=xs[:, j, :], in_=xt[:, j, :], func=AF.Copy, scale=rstd[:, j:j + 1]),
                     r=['xt', 'rstd'], w=[('xs', j)])
            if next_rows is not None:
                P.dma('act', xt[:, 0:nblk, :], next_rows.rearrange("(j p) d -> p j d", p=128), w=['xt'])
            for dc in range(8):
                half = dc % 2
                for j in range(nblk):
                    P.op('pe', lambda e, j=j, dc=dc, half=half: e.transpose(PTs[half][:, j * 128:(j + 1) * 128],
                                                                            xs[:, j, dc * 128:(dc + 1) * 128], identb[:]),
                         r=[('xs', j), 'identb'], w=[('pt', half)])
                P.op('dve', lambda e, dc=dc, half=half: e.tensor_scalar(out=xnT[:, dc, 0:nblk * 128], in0=PTs[half][:, 0:nblk * 128],
                                                                         scalar1=vec[:, gcol + dc:gcol + dc + 1], scalar2=None, op0=ALU.mult),
                     r=[('pt', half)], w=[('xnT', dc)])

        def alloc_front(ph, nblk):
            S = {}
            S['xt'] = ph.enter_context(_sbuf("xt", [128, nblk, 1024], F32))
            S['xs'] = ph.enter_context(_sbuf("xs", [128, nblk, 1024], BF16))
            S['xnT'] = ph.enter_context(_sbuf("xnT", [128, 8, nblk * 128], BF16))
            S['junk'] = ph.enter_context(_sbuf("junk", [128, 1024], BF16))
            for nm in ('ss', 'ms', 'sq', 'rstd'):
                S[nm] = ph.enter_context(_sbuf(nm, [128, 4], F32))
            return S

        def load_w_bf16(dst, src, row0, nrows_chunks, col0, ncols, key):
            for c in range(nrows_chunks):
                for s0 in range(0, ncols, 1024):
                    n = min(1024, ncols - s0)
                    P.dma('pool', dst[:, c, s0:s0 + n], src[row0 + c * 128: row0 + (c + 1) * 128, col0 + s0: col0 + s0 + n], w=[(key, c)])

        def finish_early():
            with ExitStack() as phx:
                tt = phx.enter_context(_sbuf("early", [128, 1024], F32))
                P.dma('sp', tt[:], xo[0:128, :], w=['early'])
                P.dma('sp', out_d[0:128, :], tt[:], r=['early'], w=['early_o'])
                P.barrier()
                P.emit()
            return nc

        with ExitStack() as ph:
            sb = lambda name, shape, dt=F32: ph.enter_context(_sbuf(name, shape, dt))
            S = alloc_front(ph, 4)
            wkvu = sb("wkvu", [128, 8, 2048], BF16)
            wa = sb("wa", [128, 8, 128], BF16); wx = sb("wx", [128, 8, 128], BF16)
            selt = sb("selt", [128, 8])
            uext = sb("uext", [128, 8, 515]); hT = sb("hT", [128, 8, 512]); hlast = sb("hlast", [128, 8])
            uc = [sb("uc%d" % i, [128, 512]) for i in range(8)]
            ucb = [sb("ucb%d" % i, [128, 512], BF16) for i in range(2)]
            rr = [sb("rr%d" % i, [128, 512]) for i in range(8)]
            ii = [sb("ii%d" % i, [128, 512]) for i in range(8)]
            aa = [sb("aa%d" % i, [128, 512]) for i in range(8)]
            kst = [sb("kst%d" % i, [128, 512], BF16) for i in range(2)]
            vst = [sb("vst%d" % i, [128, 512], BF16) for i in range(2)]
            hst = sb("hst", [128, 8, 128], BF16)
            load_w_bf16(wkvu, w_in, 0, 8, 512, 2048, 'wkvu')
            P.dma('pool', wa[:], lru_wa.rearrange("n c d -> c n d"), w=['wa'])
            P.dma('pool', wx[:], lru_wx.rearrange("n c d -> c n d"), w=['wx'])
            P.dma('sp', selt[:], sel_d[:, :], w=['selt'])
            P.op('pool', lambda e: e.memset(uext[:, :, 0:3], 0.0), w=[('halo', c_) for c_ in range(8)])
            for tc in range(16):
                front_end(S, xb[tc * 512:(tc + 1) * 512, :], V_GMIX, 4, 'L', next_rows=(xb[(tc + 1) * 512:(tc + 2) * 512, :] if tc < 15 else None), first=(tc == 0))
                xnT = S['xnT']
                xk = [('xnT', dc) for dc in range(8)]
                def kgroup(pair):
                    pb = PB[2 + pair % 2]
                    for dc in range(8):
                        P.op('pe', lambda e, dc=dc: e.matmul(pb[:], lhsT=wkvu[:, dc, pair * 128:(pair + 1) * 128], rhs=xnT[:, dc, :],
                                                             start=(dc == 0), stop=(dc == 7)),
                             r=[('wkvu', dc), ('xnT', dc)], w=[('pb', 2 + pair % 2)])
                    P.op('act', lambda e: e.copy(out=kst[pair % 2][:], in_=pb[:]), r=[('pb', 2 + pair % 2)], w=[('kst', pair % 2)])
                    P.dma('sp', kT_d[pair, :, tc * 512:(tc + 1) * 512], kst[pair % 2][:], r=[('kst', pair % 2)], w=[('kT_d', pair, tc)])

                def vgroup(j):
                    pb = PB[4 + j % 2]
                    for dc in range(8):
                        P.op('pe', lambda e, dc=dc: e.matmul(pb[:], lhsT=xnT[:, dc, j * 128:(j + 1) * 128], rhs=wkvu[:, dc, 512:1024],
                                                             start=(dc == 0), stop=(dc == 7)),
                             r=[('wkvu', dc), ('xnT', dc)], w=[('pb', 4 + j % 2)])
                    P.op('dve', lambda e: e.tensor_copy(out=vst[j % 2][:], in_=pb[:]), r=[('pb', 4 + j % 2)], w=[('vst', j % 2)])
                    P.dma('sp', v_d[tc * 4 + j, :, :], vst[j % 2][:], r=[('vst', j % 2)], w=[('v_d', tc * 4 + j)])

                for cc in range(8):
                    pb = PB[cc % 2]
                    for dc in range(8):
                        P.op('pe', lambda e, pb=pb, dc=dc, cc=cc: e.matmul(pb[:], lhsT=wkvu[:, dc, 1024 + cc * 128:1024 + (cc + 1) * 128], rhs=xnT[:, dc, :],
                                                                           start=(dc == 0), stop=(dc == 7)),
                             r=[('wkvu', dc), ('xnT', dc)], w=[('pb', cc % 2)])
                    P.op('act', lambda e, pb=pb, cc=cc: e.copy(out=uext[:, cc, 3:515], in_=pb[:]), r=[('pb', cc % 2)], w=[('uext', cc)])

                kvq = [lambda p_=p_: kgroup(p_) for p_ in range(4)] + [lambda j_=j_: vgroup(j_) for j_ in range(4)]

                def s1(cc):
                    kU = [('uext', cc), ('halo', cc)]
                    P.op('dve', lambda e: e.tensor_scalar(out=uc[cc][:], in0=uext[:, cc, 3:515], scalar1=vec[:, V_CW + 24 + cc:V_CW + 25 + cc],
                                                          scalar2=vec[:, V_CB + cc:V_CB + cc + 1], op0=ALU.mult, op1=ALU.add),
                         r=kU, w=[('uc', cc)])
                    for jj in range(3):
                        P.op('dve', lambda e, jj=jj: e.scalar_tensor_tensor(out=uc[cc][:], in0=uext[:, cc, jj:jj + 512],
                                                                            scalar=vec[:, V_CW + jj * 8 + cc:V_CW + jj * 8 + cc + 1],
                                                                            in1=uc[cc][:], op0=ALU.mult, op1=ALU.add),
                             r=kU + [('uc', cc)], w=[('uc', cc)])
                    P.op('pool', lambda e: e.tensor_copy(out=uext[:, cc, 0:3], in_=uext[:, cc, 512:515]), r=kU, w=kU)
                    b = cc % 2
                    P.op('act', lambda e: e.copy(out=ucb[b][:], in_=uc[cc][:]), r=[('uc', cc)], w=[('ucb', b)])
                    P.op('pe', lambda e: e.matmul(PB[2][:], lhsT=wa[:, cc, :], rhs=ucb[b][:], start=True, stop=True), r=['wa', ('ucb', b)], w=[('pb', 2)])
                    P.op('act', lambda e: e.activation(out=rr[cc][:], in_=PB[2][:], func=AF.Sigmoid, bias=vec[:, V_BA + cc:V_BA + cc + 1]),
                         r=[('pb', 2)], w=[('rr', cc)])
                    P.op('pe', lambda e: e.matmul(PB[3][:], lhsT=wx[:, cc, :], rhs=ucb[b][:], start=True, stop=True), r=['wx', ('ucb', b)], w=[('pb', 3)])
                    P.op('act', lambda e: e.activation(out=ii[cc][:], in_=PB[3][:], func=AF.Sigmoid, bias=vec[:, V_BX + cc:V_BX + cc + 1]),
                         r=[('pb', 3)], w=[('ii', cc)])

                def s2(cc):
                    P.op('act', lambda e: e.activation(out=aa[cc][:], in_=rr[cc][:], func=AF.Exp, scale=cL[:, cc:cc + 1]), r=[('rr', cc), 'cL'], w=[('aa', cc)])
                    P.op('pool', lambda e: e.tensor_tensor(out=rr[cc][:], in0=aa[cc][:], in1=aa[cc][:], op=ALU.mult), r=[('aa', cc)], w=[('rr', cc)])
                    P.op('pool', lambda e: e.tensor_tensor(out=ii[cc][:], in0=ii[cc][:], in1=uc[cc][:], op=ALU.mult), r=[('ii', cc), ('uc', cc)], w=[('ii', cc)])

                def s3(cc):
                    P.op('act', lambda e: e.activation(out=rr[cc][:], in_=rr[cc][:], func=AF.Sqrt, scale=-1.0, bias=1.0), r=[('rr', cc)], w=[('rr', cc)])
                    P.op('pool', lambda e: e.tensor_tensor(out=ii[cc][:], in0=ii[cc][:], in1=rr[cc][:], op=ALU.mult), r=[('ii', cc), ('rr', cc)], w=[('ii', cc)])

                def s4(cc):
                    if tc == 0:
                        P.op('dve', lambda e: e.tensor_tensor_scan(out=hT[:, cc, :], data0=aa[cc][:], data1=ii[cc][:], initial=0.0, op0=ALU.mult, op1=ALU.add),
                             r=[('aa', cc), ('ii', cc)], w=[('hT', cc)])
                    else:
                        P.op('dve', lambda e: e.tensor_tensor_scan(out=hT[:, cc, :], data0=aa[cc][:], data1=ii[cc][:], initial=hlast[:, cc:cc + 1],
                                                                   op0=ALU.mult, op1=ALU.add),
                             r=[('aa', cc), ('ii', cc), ('hlast', cc)], w=[('hT', cc)])
                    P.op('dve', lambda e: e.tensor_copy(out=hlast[:, cc:cc + 1], in_=hT[:, cc, 511:512]), r=[('hT', cc)], w=[('hlast', cc)])
                    so = 4 * (tc % 2)
                    P.op('dve', lambda e: e.tensor_scalar(out=hst[:, cc, :], in0=hT[:, cc, 0:128], scalar1=selt[:, so:so + 1], scalar2=None, op0=ALU.mult),
                         r=[('hT', cc), 'selt'], w=[('hst', cc)])
                    for pp in range(1, 4):
                        P.op('dve', lambda e, pp=pp: e.scalar_tensor_tensor(out=hst[:, cc, :], in0=hT[:, cc, pp * 128:(pp + 1) * 128],
                                                                            scalar=selt[:, so + pp:so + pp + 1], in1=hst[:, cc, :],
                                                                            op0=ALU.mult, op1=ALU.add),
                             r=[('hT', cc), ('hst', cc)], w=[('hst', cc)])

                h0 = range(0, 4); h1 = range(4, 8)
                for cc in h0:
                    s1(cc); kvq[cc]()
                for cc in h0:
                    s2(cc)
                for cc in h1:
                    s1(cc); kvq[cc]()
                for cc in h0:
                    s3(cc)
                for cc in h0:
                    s4(cc)
                for cc in h1:
                    s2(cc)
                for cc in h1:
                    s3(cc)
                for cc in h1:
                    s4(cc)
                P.dma('sp', hown_d[:, :, tc * 128:(tc + 1) * 128], hst[:], r=[('hst', cc) for cc in range(8)], w=[('hown_d', tc)])
            P.barrier()
            P.emit()

        if STOP == 1:
            return finish_early()
        with ExitStack() as ph:
            sb = lambda name, shape, dt=F32: ph.enter_context(_sbuf(name, shape, dt))
            S = alloc_front(ph, 4)
            wqg = sb("wqg", [128, 8, 1536], BF16); wm = sb("wm", [128, 8, 2048], BF16)
            hoc = sb("hoc", [128, 8, 512], BF16); hg = sb("hg", [128, 8, 512], BF16); gm = sb("gm", [128, 16, 512], BF16)
            qst = [sb("qst%d" % i, [128, 512], BF16) for i in range(2)]
            gl = [sb("gl%d" % i, [128, 512]) for i in range(2)]
            load_w_bf16(wqg[:, :, 0:512], w_in, 0, 8, 0, 512, 'wq')
            load_w_bf16(wqg[:, :, 512:1536], w_in, 0, 8, 2560, 1024, 'wg')
            load_w_bf16(wm, w_merge, 0, 8, 0, 2048, 'wm')
            for oc in range(4):
                front_end(S, xo[oc * 512:(oc + 1) * 512, :], V_GMIX, 4, 'O', next_rows=(xo[(oc + 1) * 512:(oc + 2) * 512, :] if oc < 3 else None), first=(oc == 0))
                xnT = S['xnT']
                P.dma('sp', hoc[:], hown_d[:, :, oc * 512:(oc + 1) * 512], w=['hoc'])
                for pair in range(4):
                    pb = PB[2 + pair % 2]
                    for dc in range(8):
                        P.op('pe', lambda e, pb=pb, dc=dc, pair=pair: e.matmul(pb[:], lhsT=wqg[:, dc, pair * 128:(pair + 1) * 128], rhs=xnT[:, dc, :],
                                                                               start=(dc == 0), stop=(dc == 7)),
                             r=[('wq', dc), ('xnT', dc)], w=[('pb', 2 + pair % 2)])
                    P.op('act', lambda e, pb=pb, pair=pair: e.activation(out=qst[pair % 2][:], in_=pb[:], func=AF.Copy, scale=0.125),
                         r=[('pb', 2 + pair % 2)], w=[('qst', pair % 2)])
                    P.dma('sp', qT_d[pair, :, oc * 512:(oc + 1) * 512], qst[pair % 2][:], r=[('qst', pair % 2)], w=[('qT_d', pair, oc)])
                for cc in range(8):
                    pb = PB[cc % 2]
                    for dc in range(8):
                        P.op('pe', lambda e, pb=pb, dc=dc, cc=cc: e.matmul(pb[:], lhsT=wqg[:, dc, 512 + cc * 128:512 + (cc + 1) * 128], rhs=xnT[:, dc, :],
                                                                           start=(dc == 0), stop=(dc == 7)),
                             r=[('wg', dc), ('xnT', dc)], w=[('pb', cc % 2)])
                    P.op('act', lambda e, pb=pb, cc=cc: e.activation(out=gl[cc % 2][:], in_=pb[:], func=AF.Gelu), r=[('pb', cc % 2)], w=[('gl', cc % 2)])
                    P.op('dve', lambda e, cc=cc: e.tensor_tensor(out=hg[:, cc, :], in0=gl[cc % 2][:], in1=hoc[:, cc, :], op=ALU.mult),
                         r=[('gl', cc % 2), 'hoc'], w=['hg'])
                P.dma('sp', hg_d[:, :, oc * 512:(oc + 1) * 512], hg[:], r=['hg'], w=[('hg_d', oc)])
                for c in range(16):
                    pb = PB[4 + c % 2]
                    for dc in range(8):
                        P.op('pe', lambda e, pb=pb, dc=dc, c=c: e.matmul(pb[:], lhsT=wm[:, dc, c * 128:(c + 1) * 128], rhs=xnT[:, dc, :],
                                                                         start=(dc == 0), stop=(dc == 7)),
                             r=[('wm', dc), ('xnT', dc)], w=[('pb', 4 + c % 2)])
                    P.op('act', lambda e, pb=pb, c=c: e.activation(out=gm[:, c, :], in_=pb[:], func=AF.Sigmoid, bias=vec[:, V_BM + c:V_BM + c + 1]),
                         r=[('pb', 4 + c % 2)], w=['gm'])
                P.dma('sp', gm_d[:, :, oc * 512:(oc + 1) * 512], gm[:], r=['gm'], w=[('gm_d', oc)])
            P.barrier()
            P.emit()

        if STOP == 2:
            return finish_early()
        with ExitStack() as ph:
            sb = lambda name, shape, dt=F32: ph.enter_context(_sbuf(name, shape, dt))
            kT = sb("kT", [128, 4, 8192], BF16); vv = sb("vv", [128, 64, 512], BF16); qT = sb("qTp", [128, 8, 2048], BF16)
            maskt = sb("maskt", [128, 1024]); ntri = sb("ntri", [128, 128]); ntrib = sb("ntrib", [128, 128], BF16)
            ones2 = sb("ones2", [128, 2]); ones2b = sb("ones2b", [128, 2], BF16)
            e_t = [sb("e_t%d" % i, [128, 512]) for i in range(2)]
            sp_t = [sb("sp_t%d" % i, [128, 512]) for i in range(2)]
            sphi = [sb("sphi%d" % i, [128, 512], BF16) for i in range(4)]
            splo = [sb("splo%d" % i, [128, 512], BF16) for i in range(4)]
            w_t = [sb("w_t%d" % i, [128, 512], BF16) for i in range(2)]
            Xc = [sb("Xc%d" % i, [128, 4]) for i in range(3)]
            Rb = [sb("Rb%d" % i, [128, 1]) for i in range(4)]
            Eself = sb("Eself", [128, 4, 6]); Esel = sb("Esel", [128, 4, 6], BF16)
            f_t = [sb("f_t%d" % i, [128, 4]) for i in range(3)]
            oacc = [sb("oacc%d" % i, [128, 64]) for i in range(2)]
            att = [sb("att%d" % i, [128, 512], BF16) for i in range(2)]
            attT = [sb("attT%d" % i, [128, 4, 128], BF16) for i in range(2)]
            P.op('pool', lambda e: e.memset(qT[:], 0.0), w=[('qT', h_) for h_ in range(8)])
            for pair in range(4):
                for hh in range(2):
                    P.dma('sp', kT[:, pair, hh * 4096:(hh + 1) * 4096], kT_d[pair, :, hh * 4096:(hh + 1) * 4096], w=[('kT', pair, hh)])
                for half in range(2):
                    P.dma('sp', qT[half * 64:(half + 1) * 64, 2 * pair + half, :], qT_d[pair, half * 64:(half + 1) * 64, :], w=[('qT', 2 * pair + half)])
            for b8 in range(8):
                P.dma('sp', vv[:, b8 * 8:(b8 + 1) * 8, :], v_d[b8 * 8:(b8 + 1) * 8, :, :].rearrange("b p c -> p b c"), w=[('vv', b8)])
            P.dma('sp', maskt[:], masks_d[:, :], w=['maskt'])
            P.dma('sp', ntri[:], ntri_d[:, :], w=['ntri'])
            P.op('dve', lambda e: e.tensor_copy(out=ntrib[:], in_=ntri[:]), r=['ntri'], w=['ntrib'])
            P.op('pool', lambda e: e.memset(ones2[:], 1.0), w=['ones2'])
            P.op('dve', lambda e: e.tensor_copy(out=ones2b[:], in_=ones2[:]), r=['ones2'], w=['ones2b'])
            P.op('pool', lambda e: e.memset(Eself[:], 0.0), w=['Eself'])
            for s_ in range(4):
                if s_ >= 1:
                    P.op('pool', lambda e, s_=s_: e.memset(Eself[:, s_, 0:s_], 1.0), r=['Eself'], w=['Eself'])
                P.op('pool', lambda e, s_=s_: e.memset(Eself[:, s_, 4:5], 1.0), r=['Eself'], w=['Eself'])
            P.op('dve', lambda e: e.tensor_copy(out=Esel[:], in_=Eself[:]), r=['Eself'], w=['Esel'])

            groups = []
            for slot in range(16):
                ng = (slot // 2) * 2 + 1 + (slot % 2)
                for h in range(8):
                    for g in range(ng):
                        groups.append((slot, h, g, ng))
            NG = len(groups)

            def zmm(gi, bank, closed):
                slot, h, g, ng = groups[gi]
                pair = h // 2
                base = 4 * ng - 4 * (g + 1)
                for s in range(4):
                    kb = base + s
                    st = True if closed else (s == 0)
                    sp_ = True if closed else False
                    P.op('pe', lambda e, s=s, kb=kb, pair=pair, h=h, slot=slot, st=st, sp_=sp_: e.matmul(
                        PB[bank][:, s * 128:(s + 1) * 128], lhsT=kT[:, pair, kb * 128:(kb + 1) * 128],
                        rhs=qT[:, h, slot * 128:(slot + 1) * 128], start=st, stop=sp_),
                        r=[('kT', pair, kb // 32), ('qT', h)], w=[('pb', bank)])

            def stageA(gi):
                slot, h, g, ng = groups[gi]
                za = gi % 2
                zmm(gi, za, True)
                P.op('act', lambda e: e.activation(out=e_t[gi % 2][:], in_=PB[za][:], func=AF.Exp), r=[('pb', za)], w=[('e_t', gi % 2)])
                P.op('act', lambda e: e.activation(out=sp_t[gi % 2][:], in_=e_t[gi % 2][:], func=AF.Ln, bias=1.0), r=[('e_t', gi % 2)], w=[('sp_t', gi % 2)])
                if g == 0:
                    mo = 512 * (slot % 2)
                    P.op('dve', lambda e, mo=mo: e.tensor_tensor(out=sp_t[gi % 2][:], in0=sp_t[gi % 2][:], in1=maskt[:, mo:mo + 512], op=ALU.mult),
                         r=[('sp_t', gi % 2), 'maskt'], w=[('sp_t', gi % 2)])
                P.op('dve', lambda e: e.tensor_copy(out=sphi[gi % 4][:], in_=sp_t[gi % 2][:]), r=[('sp_t', gi % 2)], w=[('sphi', gi % 4)])
                P.op('pool', lambda e: e.tensor_tensor(out=splo[gi % 4][:], in0=sp_t[gi % 2][:], in1=sphi[gi % 4][:], op=ALU.subtract),
                     r=[('sp_t', gi % 2), ('sphi', gi % 4)], w=[('splo', gi % 4)])

            def stageB(gi):
                slot, h, g, ng = groups[gi]
                ab = 2 + gi % 2
                zmm(gi, ab, False)
                P.op('pe', lambda e: e.matmul(PB[ab][:], lhsT=ntrib[:], rhs=sphi[gi % 4][:], start=False, stop=False),
                     r=[('sphi', gi % 4), 'ntrib'], w=[('pb', ab)])
                P.op('pe', lambda e: e.matmul(PB[ab][:], lhsT=ntrib[:], rhs=splo[gi % 4][:], start=False, stop=True),
                     r=[('splo', gi % 4), 'ntrib'], w=[('pb', ab)])
                P.op('act', lambda e: e.activation(out=w_t[gi % 2][:], in_=PB[ab][:], func=AF.Exp), r=[('pb', ab)], w=[('w_t', gi % 2)])
                for s in range(4):
                    P.op('pe', lambda e, s=s: e.matmul(PB[4][:, 0:6], lhsT=sphi[gi % 4][:, s * 128:(s + 1) * 128], rhs=Esel[:, s, :], start=(s == 0), stop=False),
                         r=[('sphi', gi % 4), 'Esel'], w=[('pb', 4)])
                    P.op('pe', lambda e, s=s: e.matmul(PB[4][:, 0:6], lhsT=splo[gi % 4][:, s * 128:(s + 1) * 128], rhs=Esel[:, s, :], start=False, stop=(s == 3)),
                         r=[('splo', gi % 4), 'Esel'], w=[('pb', 4)])
                if g == 0:
                    mo = 512 * (slot % 2)
                    P.op('dve', lambda e, mo=mo: e.tensor_tensor(out=w_t[gi % 2][:], in0=w_t[gi % 2][:], in1=maskt[:, mo:mo + 512], op=ALU.mult),
                         r=[('w_t', gi % 2), 'maskt'], w=[('w_t', gi % 2)])
                R = Rb[gi % 4]; Rn = Rb[(gi + 1) % 4]
                X = Xc[gi % 3]
                if g == 0:
                    P.op('pool', lambda e, R=R: e.memset(R[:], 0.0), w=[('R', gi % 4)])
                P.op('dve', lambda e, R=R, X=X: e.tensor_scalar(out=X[:], in0=PB[4][:, 0:4], scalar1=R[:, 0:1], scalar2=None, op0=ALU.add),
                     r=[('pb', 4), ('R', gi % 4)], w=[('X', gi % 3)])
                if g != ng - 1:
                    P.op('dve', lambda e, R=R, Rn=Rn: e.tensor_scalar(out=Rn[:], in0=PB[4][:, 4:5], scalar1=R[:, 0:1], scalar2=None, op0=ALU.add),
                         r=[('pb', 4), ('R', gi % 4)], w=[('R', (gi + 1) % 4)])
                P.op('act', lambda e, X=X: e.activation(out=f_t[gi % 3][:], in_=X[:], func=AF.Exp, scale=-1.0),
                     r=[('X', gi % 3)], w=[('f_t', gi % 3)])

            def stageC(gi):
                slot, h, g, ng = groups[gi]
                base = 4 * ng - 4 * (g + 1)
                hidx = slot * 8 + h
                oa = oacc[hidx % 2]
                ok = ('oacc', hidx % 2)
                for s in range(4):
                    kb = base + s
                    P.op('pe', lambda e, s=s, kb=kb, h=h: e.matmul(PB[5][:, s * 64:(s + 1) * 64], lhsT=w_t[gi % 2][:, s * 128:(s + 1) * 128],
                                                                   rhs=vv[:, kb, h * 64:(h + 1) * 64], start=True, stop=True),
                         r=[('w_t', gi % 2), ('vv', kb // 8)], w=[('pb', 5)])
                for s in range(4):
                    if g == 0 and s == 0:
                        P.op('dve', lambda e, s=s, oa=oa: e.tensor_scalar(out=oa[:], in0=PB[5][:, s * 64:(s + 1) * 64], scalar1=f_t[gi % 3][:, s:s + 1], scalar2=None, op0=ALU.mult),
                             r=[('pb', 5), ('f_t', gi % 3)], w=[ok])
                    else:
                        P.op('dve', lambda e, s=s, oa=oa: e.scalar_tensor_tensor(out=oa[:], in0=PB[5][:, s * 64:(s + 1) * 64], scalar=f_t[gi % 3][:, s:s + 1],
                                                                                 in1=oa[:], op0=ALU.mult, op1=ALU.add),
                             r=[('pb', 5), ('f_t', gi % 3), ok], w=[ok])
                if g == ng - 1:
                    P.op('pool', lambda e, oa=oa, h=h, slot=slot: e.tensor_copy(out=att[slot % 2][:, h * 64:(h + 1) * 64], in_=oa[:]),
                         r=[ok], w=[('att', slot % 2, h)])
                    if h == 7:
                        for c4 in range(4):
                            P.op('pe', lambda e, c4=c4, slot=slot: e.transpose(PT[:, c4 * 128:(c4 + 1) * 128], att[slot % 2][:, c4 * 128:(c4 + 1) * 128], identb[:]),
                                 r=[('att', slot % 2, 2 * c4), ('att', slot % 2, 2 * c4 + 1), 'identb'], w=['ptA'])
                        P.op('dve', lambda e, slot=slot: e.tensor_copy(out=attT[slot % 2][:], in_=PT[:, 0:512].rearrange("p (c q) -> p c q", c=4)),
                             r=['ptA'], w=[('attT', slot % 2)])
                        P.dma('sp', attT_d[:, :, slot * 128:(slot + 1) * 128], attT[slot % 2][:], r=[('attT', slot % 2)], w=[('attT_d', slot)])

            for it in range(NG + 3):
                if it < NG:
                    stageA(it)
                if 0 <= it - 2 < NG:
                    stageB(it - 2)
                if 0 <= it - 3 < NG:
                    stageC(it - 3)
            P.barrier()
            P.emit()

        if STOP == 3:
            return finish_early()
        with ExitStack() as ph:
            sb = lambda name, shape, dt=F32: ph.enter_context(_sbuf(name, shape, dt))
            wao = sb("wao", [128, 4, 1024], BF16); wlo = sb("wlo", [128, 8, 1024], BF16); wo = sb("wo", [128, 8, 1024], BF16)
            attc = sb("attc", [128, 4, 512], BF16); hgc = sb("hgc", [128, 8, 512], BF16); gmc = sb("gmc", [128, 16, 512], BF16)
            t1 = [sb("t1%d" % i, [128, 512]) for i in range(2)]
            t2 = [sb("t2%d" % i, [128, 512]) for i in range(2)]
            mT = sb("mT", [128, 8, 512], BF16)
            xres = sb("xres", [128, 4, 1024]); hres = sb("hres", [128, 4, 1024])
            load_w_bf16(wao, w_att_out, 0, 4, 0, 1024, 'wao')
            load_w_bf16(wlo, w_lru_out, 0, 8, 0, 1024, 'wlo')
            load_w_bf16(wo, w_out, 0, 8, 0, 1024, 'wo')
            for oc in range(4):
                sl = slice(oc * 512, (oc + 1) * 512)
                P.dma('sp', attc[:], attT_d[:, :, sl], w=['attc'])
                P.dma('sp', hgc[:], hg_d[:, :, sl], w=['hgc'])
                P.dma('sp', gmc[:], gm_d[:, :, sl], w=['gmc'])
                P.dma('sp', xres[:], xo[sl, :].rearrange("(j p) d -> p j d", p=128), w=['xres'])
                for dmc in range(8):
                    pa = PB[(2 * dmc) % 4]; pl = PB[(2 * dmc + 1) % 4]
                    ka = ('pb', (2 * dmc) % 4); kl = ('pb', (2 * dmc + 1) % 4)
                    for cc in range(4):
                        P.op('pe', lambda e, pa=pa, cc=cc, dmc=dmc: e.matmul(pa[:], lhsT=wao[:, cc, dmc * 128:(dmc + 1) * 128], rhs=attc[:, cc, :],
                                                                             start=(cc == 0), stop=(cc == 3)), r=[('wao', cc), 'attc'], w=[ka])
                    for cc in range(8):
                        P.op('pe', lambda e, pl=pl, cc=cc, dmc=dmc: e.matmul(pl[:], lhsT=wlo[:, cc, dmc * 128:(dmc + 1) * 128], rhs=hgc[:, cc, :],
                                                                             start=(cc == 0), stop=(cc == 7)), r=[('wlo', cc), 'hgc'], w=[kl])
                    b = dmc % 2
                    P.op('dve', lambda e, pa=pa, dmc=dmc, b=b: e.tensor_tensor(out=t1[b][:], in0=pa[:], in1=gmc[:, dmc, :], op=ALU.mult), r=[ka, 'gmc'], w=[('t1', b)])
                    P.op('dve', lambda e, pl=pl, dmc=dmc, b=b: e.tensor_tensor(out=t2[b][:], in0=pl[:], in1=gmc[:, 8 + dmc, :], op=ALU.mult), r=[kl, 'gmc'], w=[('t2', b)])
                    P.op('pool', lambda e, dmc=dmc, b=b: e.tensor_tensor(out=mT[:, dmc, :], in0=t1[b][:], in1=t2[b][:], op=ALU.add),
                         r=[('t1', b), ('t2', b)], w=[('mT', dmc)])
                for j in range(4):
                    for n in range(2):
                        pi = 4 + (j * 2 + n) % 2
                        pb = PB[pi]
                        for dmc in range(8):
                            P.op('pe', lambda e, pb=pb, dmc=dmc, j=j, n=n: e.matmul(pb[:], lhsT=mT[:, dmc, j * 128:(j + 1) * 128], rhs=wo[:, dmc, n * 512:(n + 1) * 512],
                                                                                    start=(dmc == 0), stop=(dmc == 7)), r=[('mT', dmc), ('wo', dmc)], w=[('pb', pi)])
                        P.op('dve', lambda e, pb=pb, j=j, n=n: e.tensor_tensor(out=hres[:, j, n * 512:(n + 1) * 512], in0=pb[:], in1=xres[:, j, n * 512:(n + 1) * 512], op=ALU.add),
                             r=[('pb', pi), 'xres'], w=['hres'])
                P.dma('sp', h_d[sl, :].rearrange("(j p) d -> p j d", p=128), hres[:], r=['hres'], w=[('h_d', oc)])
            P.barrier()
            P.emit()

        if STOP == 4:
            return finish_early()
        with ExitStack() as ph:
            sb = lambda name, shape, dt=F32: ph.enter_context(_sbuf(name, shape, dt))
            phH = ExitStack()
            sbH = lambda name, shape, dt=F32: phH.enter_context(_sbuf(name, shape, dt))
            H = sbH("H", [128, 16, 1024])
            junkE = sbH("junkE", [128, 1024], BF16); ones1 = sbH("ones1", [1, 128])
            sm = {nm: sbH("e_" + nm, [128, 8]) for nm in ('ss', 'ms', 'sq', 'rstd', 'mx', 'negm', 'sum', 'rs')}
            xn2T = sb("xn2T", [128, 8, 2048], BF16)
            G = sb("G", [128, 16, 32])
            xsf = sb("xsf", [128, 1024]); xTf = sb("xTf", [128, 8, 128])
            wr = sb("wr", [128, 8, 32]); brt = sb("brt", [1, 32])
            lg = sb("lg", [128, 32]); mk = sb("mk", [128, 32]); ex = sb("ex", [128, 32])
            wgt = [sb("wgt%d" % i, [128, 8, 1024], BF16) for i in range(2)]
            wdt = [sb("wdt%d" % i, [128, 4, 1024], BF16) for i in range(2)]
            bdall = sb("bdall", [32, 1024]); GT = sb("GT", [32, 128])
            gc = [sb("gc%d" % i, [128, 512]) for i in range(2)]
            sg = [sb("sg%d" % i, [128, 512]) for i in range(2)]
            u1 = [sb("u1%d" % i, [128, 512]) for i in range(2)]
            actT = [sb("actT%d" % i, [128, 4, 512], BF16) for i in range(2)]
            for oc in range(4):
                P.dma('sp', H[:, oc * 4:(oc + 1) * 4, :], h_d[oc * 512:(oc + 1) * 512, :].rearrange("(j p) d -> p j d", p=128), w=[('H', oc * 4 + j) for j in range(4)])
            P.dma('sp', wr[:], w_router.rearrange("(c p) e -> p c e", p=128), w=['wr'])
            P.dma('sp', brt[:], b_router[:, :], w=['brt'])
            P.dma('sp', bdall[:], b_dn[:, :], w=['bdall'])
            P.op('pool', lambda e: e.memset(ones1[:], 1.0), w=['ones1'])
            for blk in range(16):
                P.op('act', lambda e, blk=blk: e.activation(out=junkE[:], in_=H[:, blk, :], func=AF.Square, accum_out=sm['ss'][:, 0:1]),
                     r=[('H', blk)], w=['junkE', 'ss'])
                P.op('dve', lambda e: e.tensor_scalar(out=sm['ms'][:, 0:1], in0=sm['ss'][:, 0:1], scalar1=1.0 / 1024, scalar2=EPS, op0=ALU.mult, op1=ALU.add), r=['ss'], w=['ms'])
                P.op('act', lambda e: e.activation(out=sm['sq'][:, 0:1], in_=sm['ms'][:, 0:1], func=AF.Sqrt), r=['ms'], w=['sq'])
                P.op('dve', lambda e: e.reciprocal(out=sm['rstd'][:, 0:1], in_=sm['sq'][:, 0:1]), r=['sq'], w=['rstd'])
                P.op('act', lambda e, blk=blk: e.activation(out=xsf[:], in_=H[:, blk, :], func=AF.Copy, scale=sm['rstd'][:, 0:1]), r=[('H', blk), 'rstd'], w=['xsf'])
                for dc in range(8):
                    pi = dc % 2
                    P.op('pe', lambda e, dc=dc, pi=pi: e.transpose(PB[pi][:, 0:128], xsf[:, dc * 128:(dc + 1) * 128], identf[:]), r=['xsf', 'identf'], w=[('pb', pi)])
                    P.op('dve', lambda e, dc=dc, pi=pi: e.tensor_scalar(out=xTf[:, dc, :], in0=PB[pi][:, 0:128], scalar1=vec[:, V_GMOE + dc:V_GMOE + dc + 1], scalar2=None, op0=ALU.mult),
                         r=[('pb', pi)], w=[('xTf', dc)])
                    P.op('pool', lambda e, dc=dc, blk=blk: e.tensor_copy(out=xn2T[:, dc, blk * 128:(blk + 1) * 128], in_=xTf[:, dc, :]), r=[('xTf', dc)], w=[('xn2T', blk)])
                for dc in range(8):
                    P.op('pe', lambda e, dc=dc: e.matmul(PB[2][:, 0:32], lhsT=xTf[:, dc, :], rhs=wr[:, dc, :], start=(dc == 0), stop=False),
                         r=[('xTf', dc), 'wr'], w=[('pb', 2)])
                P.op('pe', lambda e: e.matmul(PB[2][:, 0:32], lhsT=ones1[:], rhs=brt[:], start=False, stop=True), r=['ones1', 'brt'], w=[('pb', 2)])
                P.op('dve', lambda e: e.tensor_copy(out=lg[:], in_=PB[2][:, 0:32]), r=[('pb', 2)], w=['lg'])
                P.op('dve', lambda e: e.max(out=sm['mx'][:], in_=lg[:]), r=['lg'], w=['mx'])
                P.op('dve', lambda e: e.tensor_scalar(out=mk[:], in0=lg[:], scalar1=sm['mx'][:, 3:4], scalar2=None, op0=ALU.is_ge), r=['lg', 'mx'], w=['mk'])
                P.op('dve', lambda e: e.tensor_scalar(out=sm['negm'][:, 0:1], in0=sm['mx'][:, 0:1], scalar1=-1.0, scalar2=None, op0=ALU.mult), r=['mx'], w=['negm'])
                P.op('act', lambda e: e.activation(out=ex[:], in_=lg[:], func=AF.Exp, bias=sm['negm'][:, 0:1]), r=['lg', 'negm'], w=['ex'])
                P.op('dve', lambda e: e.tensor_tensor(out=ex[:], in0=ex[:], in1=mk[:], op=ALU.mult), r=['ex', 'mk'], w=['ex'])
                P.op('dve', lambda e: e.tensor_reduce(out=sm['sum'][:, 0:1], in_=ex[:], axis=AX.X, op=ALU.add), r=['ex'], w=['sum'])
                P.op('dve', lambda e: e.reciprocal(out=sm['rs'][:, 0:1], in_=sm['sum'][:, 0:1]), r=['sum'], w=['rs'])
                P.op('dve', lambda e, blk=blk: e.tensor_scalar(out=G[:, blk, :], in0=ex[:], scalar1=sm['rs'][:, 0:1], scalar2=None, op0=ALU.mult), r=['ex', 'rs'], w=[('G', blk)])
                P.op('pe', lambda e, blk=blk: e.transpose(PB[3][0:32, 0:128], G[:, blk, :], identf[:]), r=[('G', blk), 'identf'], w=[('pb', 3)])
                P.op('dve', lambda e: e.tensor_copy(out=GT[:], in_=PB[3][0:32, 0:128]), r=[('pb', 3)], w=['GT'])
                for n in range(2):
                    P.op('pe', lambda e, n=n: e.matmul(PB[4 + n][:], lhsT=GT[:], rhs=bdall[:, n * 512:(n + 1) * 512], start=True, stop=True), r=['GT', 'bdall'], w=[('pb', 4 + n)])
                    P.op('dve', lambda e, n=n, blk=blk: e.tensor_tensor(out=H[:, blk, n * 512:(n + 1) * 512], in0=PB[4 + n][:], in1=H[:, blk, n * 512:(n + 1) * 512], op=ALU.add),
                         r=[('pb', 4 + n), ('H', blk)], w=[('H', blk)])
            def load_expert(ei, hf):
                wb = (ei * 2 + hf) % 2
                for dc in range(8):
                    P.dma('pool', wgt[wb][:, dc, :].rearrange("p (a f) -> p a f", a=2),
                          w_gu[ei, dc * 128:(dc + 1) * 128, :].rearrange("p (a f) -> p a f", a=2)[:, :, hf * 512:(hf + 1) * 512], w=[('wgt', wb, dc)])
                for fc in range(4):
                    P.dma('pool', wdt[wb][:, fc, :], w_dn[ei, hf * 512 + fc * 128: hf * 512 + (fc + 1) * 128, :], w=[('wdt', wb, fc)])

            seq = [(ei, hf) for ei in range(32) for hf in range(2)]

            def GU(si, tg, ab):
                ei, hf = seq[si]
                wb = si % 2
                for fc in range(4):
                    pg = PB[(2 * fc) % 4]; pu = PB[(2 * fc + 1) % 4]
                    kg = ('pb', (2 * fc) % 4); ku = ('pb', (2 * fc + 1) % 4)
                    for dc in range(8):
                        P.op('pe', lambda e, pg=pg, dc=dc, fc=fc: e.matmul(pg[:], lhsT=wgt[wb][:, dc, fc * 128:(fc + 1) * 128], rhs=xn2T[:, dc, tg * 512:(tg + 1) * 512],
                                                                           start=(dc == 0), stop=(dc == 7)),
                             r=[('wgt', wb, dc)] + [('xn2T', tg * 4 + j) for j in range(4)], w=[kg])
                    for dc in range(8):
                        P.op('pe', lambda e, pu=pu, dc=dc, fc=fc: e.matmul(pu[:], lhsT=wgt[wb][:, dc, 512 + fc * 128:512 + (fc + 1) * 128], rhs=xn2T[:, dc, tg * 512:(tg + 1) * 512],
                                                                           start=(dc == 0), stop=(dc == 7)),
                             r=[('wgt', wb, dc)] + [('xn2T', tg * 4 + j) for j in range(4)], w=[ku])
                    b = fc % 2
                    cg = V_BGU + ei * 16 + hf * 4 + fc
                    cu = V_BGU + ei * 16 + 8 + hf * 4 + fc
                    P.op('dve', lambda e, pg=pg, b=b, cg=cg: e.tensor_scalar(out=gc[b][:], in0=pg[:], scalar1=vec[:, cg:cg + 1], scalar2=7.0, op0=ALU.add, op1=ALU.min), r=[kg], w=[('gc', b)])
                    P.op('act', lambda e, b=b: e.activation(out=sg[b][:], in_=gc[b][:], func=AF.Sigmoid, scale=1.702), r=[('gc', b)], w=[('sg', b)])
                    P.op('dve', lambda e, pu=pu, b=b, cu=cu: e.tensor_scalar(out=u1[b][:], in0=pu[:], scalar1=vec[:, cu:cu + 1], scalar2=-7.0, op0=ALU.add, op1=ALU.max), r=[ku], w=[('u1', b)])
                    P.op('dve', lambda e, b=b: e.tensor_scalar(out=u1[b][:], in0=u1[b][:], scalar1=7.0, scalar2=1.0, op0=ALU.min, op1=ALU.add), r=[('u1', b)], w=[('u1', b)])
                    P.op('pool', lambda e, b=b: e.tensor_tensor(out=gc[b][:], in0=gc[b][:], in1=sg[b][:], op=ALU.mult), r=[('gc', b), ('sg', b)], w=[('gc', b)])
                    P.op('pool', lambda e, b=b, fc=fc: e.tensor_tensor(out=actT[ab][:, fc, :], in0=gc[b][:], in1=u1[b][:], op=ALU.mult), r=[('gc', b), ('u1', b)], w=[('actT', ab, fc)])

            def DN(si, tg, ab):
                ei, hf = seq[si]
                wb = si % 2
                for j in range(4):
                    blk = tg * 4 + j
                    for n in range(2):
                        pi = 4 + (j * 2 + n) % 2
                        pb = PB[pi]
                        for fc in range(4):
                            P.op('pe', lambda e, pb=pb, fc=fc, j=j, n=n: e.matmul(pb[:], lhsT=actT[ab][:, fc, j * 128:(j + 1) * 128], rhs=wdt[wb][:, fc, n * 512:(n + 1) * 512],
                                                                                  start=(fc == 0), stop=(fc == 3)),
                                 r=[('actT', ab, fc), ('wdt', wb, fc)], w=[('pb', pi)])
                        P.op('dve', lambda e, pb=pb, blk=blk, n=n: e.scalar_tensor_tensor(out=H[:, blk, n * 512:(n + 1) * 512], in0=pb[:], scalar=G[:, blk, ei:ei + 1],
                                                                                          in1=H[:, blk, n * 512:(n + 1) * 512], op0=ALU.mult, op1=ALU.add),
                             r=[('pb', pi), ('G', blk), ('H', blk)], w=[('H', blk)])

            load_expert(0, 0)
            units = [(si, tg) for si in range(len(seq)) for tg in range(4)]
            for ui in range(len(units) + 1):
                if ui < len(units):
                    si, tg = units[ui]
                    GU(si, tg, ui % 2)
                if ui >= 1:
                    psi, ptg = units[ui - 1]
                    DN(psi, ptg, (ui - 1) % 2)
                if ui < len(units):
                    si, tg = units[ui]
                    if tg == 0 and si + 1 < len(seq):
                        load_expert(*seq[si + 1])
            if DBG:
                h2_d = dscr("h2_d", [2048, 1024], F32)
                for oc in range(4):
                    P.dma('sp', h2_d[oc * 512:(oc + 1) * 512, :].rearrange("(j p) d -> p j d", p=128), H[:, oc * 4:(oc + 1) * 4, :],
                          r=[('H', oc * 4 + j) for j in range(4)], w=[('h2_d', oc)])
                g_d = dscr("g_d", [128, 16, 32], F32)
                P.dma('sp', g_d[:, :, :], G[:], r=[('G', b_) for b_ in range(16)], w=['g_d'])
            P.barrier()
            P.emit()

            ph.close()
            if STOP == 5:
                phH.close()
                return finish_early()
            with ExitStack() as ph2:
                sb2 = lambda name, shape, dt=F32: ph2.enter_context(_sbuf(name, shape, dt))
                wpg = sb2("wpg", [128, 8, 1024], BF16); wpp = sb2("wpp", [128, 2, 1024], BF16)
                bpg = sb2("bpg", [1, 1024]); gpo = sb2("gpo", [128, 1024]); gfi = sb2("gfi", [128, 1024])
                hs = sb2("hs", [128, 1024], BF16); hnT = sb2("hnT", [128, 8, 128], BF16)
                pt_ = sb2("pt_", [128, 256]); ptb = sb2("ptb_", [128, 256], BF16); pT = sb2("pT", [128, 2, 128], BF16)
                gate = sb2("gate", [128, 1024]); plr = sb2("plr", [128, 1024]); ot = [sb2("ot%d" % i, [128, 1024]) for i in range(2)]
                load_w_bf16(wpg, w_plg, 0, 8, 0, 1024, 'wpg')
                load_w_bf16(wpp, w_plp, 0, 2, 0, 1024, 'wpp')
                P.dma('sp', bpg[:], b_plg[:, :], w=['bpg'])
                P.dma('sp', gpo[:], g_post.partition_broadcast(128), w=['gpo'])
                P.dma('sp', gfi[:], g_fin.partition_broadcast(128), w=['gfi'])

                def rstd_blk(src_ap, rk, col):
                    P.op('act', lambda e: e.activation(out=junkE[:], in_=src_ap, func=AF.Square, accum_out=sm['ss'][:, col:col + 1]), r=rk, w=['junkE', ('ss', col)])
                    P.op('dve', lambda e: e.tensor_scalar(out=sm['ms'][:, col:col + 1], in0=sm['ss'][:, col:col + 1], scalar1=1.0 / 1024, scalar2=EPS, op0=ALU.mult, op1=ALU.add),
                         r=[('ss', col)], w=[('ms', col)])
                    P.op('act', lambda e: e.activation(out=sm['sq'][:, col:col + 1], in_=sm['ms'][:, col:col + 1], func=AF.Sqrt), r=[('ms', col)], w=[('sq', col)])
                    P.op('dve', lambda e: e.reciprocal(out=sm['rstd'][:, col:col + 1], in_=sm['sq'][:, col:col + 1]), r=[('sq', col)], w=[('rstd', col)])

                for blk in range(16):
                    Hb = H[:, blk, :]
                    rstd_blk(Hb, [('H', blk)], 1)
                    P.op('act', lambda e, Hb=Hb: e.activation(out=hs[:], in_=Hb, func=AF.Copy, scale=sm['rstd'][:, 1:2]), r=[('H', blk), ('rstd', 1)], w=['hs'])
                    for dc in range(8):
                        P.op('pe', lambda e, dc=dc: e.transpose(PT[:, dc * 128:(dc + 1) * 128], hs[:, dc * 128:(dc + 1) * 128], identb[:]), r=['hs', 'identb'], w=['ptP'])
                    for dc in range(8):
                        P.op('dve', lambda e, dc=dc: e.tensor_scalar(out=hnT[:, dc, :], in0=PT[:, dc * 128:(dc + 1) * 128], scalar1=vec[:, V_GPL + dc:V_GPL + dc + 1], scalar2=None, op0=ALU.mult),
                             r=['ptP'], w=[('hnT', dc)])
                    for n in range(2):
                        pb = PB[n]
                        for dc in range(8):
                            P.op('pe', lambda e, pb=pb, dc=dc, n=n: e.matmul(pb[:], lhsT=hnT[:, dc, :], rhs=wpg[:, dc, n * 512:(n + 1) * 512], start=(dc == 0), stop=False),
                                 r=[('hnT', dc), ('wpg', dc)], w=[('pb', n)])
                        P.op('pe', lambda e, pb=pb, n=n: e.matmul(pb[:], lhsT=ones1[:], rhs=bpg[:, n * 512:(n + 1) * 512], start=False, stop=True), r=['ones1', 'bpg'], w=[('pb', n)])
                        P.op('act', lambda e, pb=pb, n=n: e.activation(out=gate[:, n * 512:(n + 1) * 512], in_=pb[:], func=AF.Sigmoid), r=[('pb', n)], w=['gate'])
                    P.dma('sp', pt_[:], po[blk * 128:(blk + 1) * 128, :], w=['pt_'])
                    P.op('pool', lambda e: e.tensor_copy(out=ptb[:], in_=pt_[:]), r=['pt_'], w=['ptb'])
                    for kc in range(2):
                        P.op('pe', lambda e, kc=kc: e.transpose(PTs[1][:, kc * 128:(kc + 1) * 128], ptb[:, kc * 128:(kc + 1) * 128], identb[:]), r=['ptb', 'identb'], w=['ptP1'])
                    P.op('dve', lambda e: e.tensor_copy(out=pT[:], in_=PTs[1][:, 0:256].rearrange("p (c q) -> p c q", c=2)), r=['ptP1'], w=['pT'])
                    for n in range(2):
                        pb = PB[2 + n]
                        for kc in range(2):
                            P.op('pe', lambda e, pb=pb, kc=kc, n=n: e.matmul(pb[:], lhsT=pT[:, kc, :], rhs=wpp[:, kc, n * 512:(n + 1) * 512], start=(kc == 0), stop=(kc == 1)),
                                 r=['pT', ('wpp', kc)], w=[('pb', 2 + n)])
                        P.op('act', lambda e, pb=pb, n=n: e.copy(out=plr[:, n * 512:(n + 1) * 512], in_=pb[:]), r=[('pb', 2 + n)], w=['plr'])
                    rstd_blk(plr[:], ['plr'], 2)
                    P.op('dve', lambda e: e.scalar_tensor_tensor(out=plr[:], in0=plr[:], scalar=sm['rstd'][:, 2:3], in1=gpo[:], op0=ALU.mult, op1=ALU.mult),
                         r=['plr', ('rstd', 2), 'gpo'], w=['plr'])
                    P.op('pool', lambda e: e.tensor_tensor(out=plr[:], in0=plr[:], in1=gate[:], op=ALU.mult), r=['plr', 'gate'], w=['plr'])
                    P.op('pool', lambda e, Hb=Hb: e.tensor_tensor(out=Hb, in0=Hb, in1=plr[:], op=ALU.add), r=['plr', ('H', blk)], w=[('H', blk)])
                    rstd_blk(Hb, [('H', blk)], 3)
                    o = ot[blk % 2]
                    P.op('dve', lambda e, Hb=Hb, o=o: e.scalar_tensor_tensor(out=o[:], in0=Hb, scalar=sm['rstd'][:, 3:4], in1=gfi[:], op0=ALU.mult, op1=ALU.mult),
                         r=[('H', blk), ('rstd', 3), 'gfi'], w=[('ot', blk % 2)])
                    P.dma('sp', out_d[blk * 128:(blk + 1) * 128, :], o[:], r=[('ot', blk % 2)], w=[('out', blk)])
                P.barrier()
                P.emit()
            phH.close()
    return nc


def _fm(v, n):
    return np.ascontiguousarray(np.asarray(v, np.float32).reshape(n, 128).T)


def kernel(**inp):
    f = lambda k: np.asarray(inp[k], np.float32)
    x = f("x"); p = f("p")[0]
    vecs = np.zeros((128, NV), np.float32)
    vecs[:, V_GMIX:V_GMIX + 8] = _fm(f("norm_mix_g")[0], 8)
    vecs[:, V_GMOE:V_GMOE + 8] = _fm(f("norm_moe_g")[0], 8)
    vecs[:, V_GPL:V_GPL + 8] = _fm(f("norm_pl_g")[0], 8)
    cw = f("conv_w")[0]
    for j in range(4):
        vecs[:, V_CW + j * 8:V_CW + (j + 1) * 8] = _fm(cw[j], 8)
    vecs[:, V_CB:V_CB + 8] = _fm(f("conv_b")[0], 8)
    vecs[:, V_BA:V_BA + 8] = _fm(f("lru_ba")[0], 8)
    vecs[:, V_BX:V_BX + 8] = _fm(f("lru_bx")[0], 8)
    vecs[:, V_LAM:V_LAM + 8] = _fm(f("lru_lambda")[0], 8)
    vecs[:, V_BM:V_BM + 16] = _fm(f("b_merge")[0], 16)
    bgu = f("b_gate_up")[0]
    for e in range(32):
        vecs[:, V_BGU + e * 16:V_BGU + (e + 1) * 16] = _fm(bgu[e], 16)
    ident = np.eye(128, dtype=np.float32)
    ntri = -np.tril(np.ones((128, 128), np.float32))
    tri_mask = np.triu(np.ones((128, 128), np.float32), 1)
    common = {
        "ident": ident, "ntri": ntri, "vecs": vecs,
        "w_in": f("w_in")[0], "lru_wa": f("lru_wa")[0], "lru_wx": f("lru_wx")[0],
        "w_att_out": f("w_att_out")[0], "w_lru_out": f("w_lru_out")[0], "w_merge": f("w_merge")[0], "w_out": f("w_out")[0],
        "w_router": f("w_router")[0], "b_router": f("b_router")[0].reshape(1, 32),
        "w_gate_up": f("w_gate_up")[0], "w_down": f("w_down")[0], "b_down": f("b_down")[0],
        "w_pl_gate": f("w_pl_gate")[0], "b_pl_gate": f("b_pl_gate")[0].reshape(1, 1024), "w_pl_proj": f("w_pl_proj")[0],
        "g_post": f("norm_pl_post_g")[0].reshape(1, 1024), "g_fin": f("norm_final_g").reshape(1, 1024),
    }
    in_maps = []
    own_rows = []
    for c in range(8):
        b, r = c // 4, c % 4
        blks = []
        for i in range(8):
            blks += [8 * i + r, 8 * i + 7 - r]
        rows = np.concatenate([np.arange(bk * 128, (bk + 1) * 128) for bk in blks])
        own_rows.append((b, rows))
        masks = np.zeros((128, 8, 128), np.float32)
        sel = np.zeros((128, 8), np.float32)
        for pp in range(4):
            masks[:, pp, :] = 1.0 if pp < r else (tri_mask if pp == r else 0.0)
            masks[:, 4 + pp, :] = 1.0 if pp < 3 - r else (tri_mask if pp == 3 - r else 0.0)
        sel[:, r] = 1.0
        sel[:, 4 + 3 - r] = 1.0
        m = dict(common)
        m["xb"] = np.ascontiguousarray(x[b])
        m["xo"] = np.ascontiguousarray(x[b][rows])
        m["po"] = np.ascontiguousarray(p[b][rows])
        m["masks"] = masks.reshape(128, 1024)
        m["sel"] = sel
        in_maps.append(m)
    nc = build_program()
    res = run_bass_kernel_spmd(nc, in_maps, core_ids=list(range(8)))
    if DBG:
        LAST['res'] = res.results
        LAST['own_rows'] = own_rows
    out = np.zeros((2, 8192, 1024), np.float32)
    for c in range(8):
        b, rows = own_rows[c]
        out[b, rows] = np.asarray(res.results[c]["out"], np.float32)
    return out
```

```python
import numpy as np
import concourse.bass as bass
import concourse.mybir as mybir
from concourse.bass_utils import run_bass_kernel_spmd
from contextlib import ExitStack
import os
STOP = int(os.environ.get('KSTOP', '99'))
DBG = os.environ.get('KDBG', '') != ''
LAST = {}

F32 = mybir.dt.float32
BF16 = mybir.dt.bfloat16
AF = mybir.ActivationFunctionType
ALU = mybir.AluOpType
AX = mybir.AxisListType

ENG = ['pe', 'act', 'dve', 'pool', 'sp']
EOBJ = {'pe': 'tensor', 'act': 'scalar', 'dve': 'vector', 'pool': 'gpsimd', 'sp': 'sync'}
NDS = 8
EPS = 1e-6


class Prog:
    def __init__(self, nc, es):
        self.nc = nc
        self.ops = {e: [] for e in ENG}
        self.cnt = {e: 0 for e in ENG}
        self.sem = {e: es.enter_context(nc.semaphore("s_" + e)) for e in ENG}
        self.seen = {e: {} for e in ENG}
        self.dq = ('sp', 'pool', 'act')
        self.dsem = {q: [es.enter_context(nc.semaphore("d_%s%d" % (q, i))) for i in range(NDS)] for q in self.dq}
        self.dcnt = {q: 0 for q in self.dq}
        self.last_w = {}
        self.readers = {}

    def need(self, eng, tok):
        kind, src, n = tok
        if kind == 'e':
            if src == eng and eng == 'pe':
                return
            key = ('e', src)
            val = n
            sem = self.sem[src]
        else:
            slot = n % NDS
            key = ('d', src, slot)
            val = 16 * (n // NDS + 1)
            sem = self.dsem[src][slot]
        if self.seen[eng].get(key, 0) >= val:
            return
        self.seen[eng][key] = val
        self.ops[eng].append(('wait', sem, val))

    def _deps(self, eng, r, w):
        toks = []
        for k in r:
            if k in self.last_w:
                toks.append(self.last_w[k])
        for k in w:
            if k in self.last_w:
                toks.append(self.last_w[k])
            toks.extend(self.readers.get(k, {}).values())
        for t in toks:
            self.need(eng, t)

    def _mark(self, tok, r, w):
        for k in r:
            d = self.readers.setdefault(k, {})
            if tok[0] == 'e':
                d[('e', tok[1])] = tok
            else:
                d[tok] = tok
        for k in w:
            self.last_w[k] = tok
            self.readers[k] = {}

    def op(self, eng, fn, r=(), w=()):
        self._deps(eng, r, w)
        n = self.cnt[eng] + 1
        self.cnt[eng] = n
        self.ops[eng].append(('op', fn))
        self._mark(('e', eng, n), r, w)

    def dma(self, q, out_ap, in_ap, r=(), w=()):
        self._deps(q, r, w)
        i = self.dcnt[q]
        self.dcnt[q] = i + 1
        if i >= NDS:
            self.need(q, ('d', q, i - NDS))
        self.ops[q].append(('dma', out_ap, in_ap, i % NDS))
        self._mark(('d', q, i), r, w)

    def barrier(self):
        for e in ENG:
            for q in self.dq:
                for i in range(max(0, self.dcnt[q] - NDS), self.dcnt[q]):
                    self.need(e, ('d', q, i))
            for f in ENG:
                if f != e and self.cnt[f] > 0:
                    self.need(e, ('e', f, self.cnt[f]))
        self.last_w = {}
        self.readers = {}

    def emit(self):
        nc = self.nc
        with nc.Block() as block:
            for e in ENG:
                items = self.ops[e]

                def body(eng, e=e, items=items):
                    for item in items:
                        if item[0] == 'wait':
                            eng.wait_ge(item[1], item[2])
                        elif item[0] == 'op':
                            item[1](eng).then_inc(self.sem[e], 1)
                        else:
                            _, o, i_, slot = item
                            eng.dma_start(out=o, in_=i_).then_inc(self.dsem[e][slot], 16)
                getattr(block, EOBJ[e])(body)
        self.ops = {e: [] for e in ENG}


V_GMIX, V_GMOE, V_GPL, V_CW, V_CB, V_BA, V_BX, V_LAM, V_BM, V_BGU = 0, 8, 16, 24, 56, 64, 72, 80, 88, 104
NV = 104 + 512


def build_program():
    nc = bass.Bass("TRN2", target_bir_lowering=False)

    def din(name, shape, dt=F32):
        return nc.dram_tensor(name, shape, dt, kind="ExternalInput").ap()

    def dscr(name, shape, dt):
        return nc.dram_tensor(name, shape, dt, kind=("ExternalOutput" if DBG else "Internal")).ap()

    xb = din("xb", [8192, 1024]); xo = din("xo", [2048, 1024]); po = din("po", [2048, 256])
    masks_d = din("masks", [128, 1024]); sel_d = din("sel", [128, 8])
    ident_d = din("ident", [128, 128]); ntri_d = din("ntri", [128, 128]); vecs_d = din("vecs", [128, NV])
    w_in = din("w_in", [1024, 3584]); lru_wa = din("lru_wa", [8, 128, 128]); lru_wx = din("lru_wx", [8, 128, 128])
    w_att_out = din("w_att_out", [512, 1024]); w_lru_out = din("w_lru_out", [1024, 1024])
    w_merge = din("w_merge", [1024, 2048]); w_out = din("w_out", [1024, 1024])
    w_router = din("w_router", [1024, 32]); b_router = din("b_router", [1, 32])
    w_gu = din("w_gate_up", [32, 1024, 2048]); w_dn = din("w_down", [32, 1024, 1024]); b_dn = din("b_down", [32, 1024])
    w_plg = din("w_pl_gate", [1024, 1024]); b_plg = din("b_pl_gate", [1, 1024]); w_plp = din("w_pl_proj", [256, 1024])
    g_post = din("g_post", [1, 1024]); g_fin = din("g_fin", [1, 1024])
    out_d = nc.dram_tensor("out", [2048, 1024], F32, kind="ExternalOutput").ap()

    kT_d = dscr("kT_d", [4, 128, 8192], BF16); v_d = dscr("v_d", [64, 128, 512], BF16)
    hown_d = dscr("hown_d", [128, 8, 2048], BF16); qT_d = dscr("qT_d", [4, 128, 2048], BF16)
    hg_d = dscr("hg_d", [128, 8, 2048], BF16); gm_d = dscr("gm_d", [128, 16, 2048], BF16)
    attT_d = dscr("attT_d", [128, 4, 2048], BF16); h_d = dscr("h_d", [2048, 1024], F32)

    with ExitStack() as es:
        P = Prog(nc, es)
        _uid = [0]
        _orig_sbuf = nc.sbuf_tensor

        def _sbuf(name, shape, dt):
            _uid[0] += 1
            return _orig_sbuf("%s_%d" % (name, _uid[0]), shape, dt)
        PB = [es.enter_context(nc.psum_tensor("pb%d" % i, [128, 512], F32)) for i in range(6)]
        PTs = [es.enter_context(nc.psum_tensor("ptb%d" % i, [128, 1024], BF16)) for i in range(2)]
        PT = PTs[0]

        def sbp(name, shape, dt=F32):
            return es.enter_context(_sbuf(name, shape, dt))
        vec = sbp("vec", [128, NV]); identf = sbp("identf", [128, 128]); identb = sbp("identb", [128, 128], BF16)
        cL = sbp("cL", [128, 8]); tmp8 = sbp("tmp8", [128, 8])
        P.dma('sp', vec[:], vecs_d[:, :], w=['vec'])
        P.dma('sp', identf[:], ident_d[:, :], w=['identf'])
        P.op('dve', lambda e: e.tensor_copy(out=identb[:], in_=identf[:]), r=['identf'], w=['identb'])
        P.op('act', lambda e: e.activation(out=tmp8[:], in_=vec[:, V_LAM:V_LAM + 8], func=AF.Exp, scale=-1.0), r=['vec'], w=['tmp8'])
        P.op('act', lambda e: e.activation(out=tmp8[:], in_=tmp8[:], func=AF.Ln, bias=1.0), r=['tmp8'], w=['tmp8'])
        P.op('dve', lambda e: e.tensor_scalar(out=cL[:], in0=tmp8[:], scalar1=-8.0, scalar2=None, op0=ALU.mult), r=['tmp8'], w=['cL'])
        P.emit()

        def front_end(S, src_rows, gcol, nblk, tag, next_rows=None, first=True):
            xt, xs, xnT, junk, ss, ms, sq, rstd = S['xt'], S['xs'], S['xnT'], S['junk'], S['ss'], S['ms'], S['sq'], S['rstd']
            if first:
                P.dma('sp', xt[:, 0:nblk, :], src_rows.rearrange("(j p) d -> p j d", p=128), w=['xt'])
            for j in range(nblk):
                P.op('act', lambda e, j=j: e.activation(out=junk[:], in_=xt[:, j, :], func=AF.Square, accum_out=ss[:, j:j + 1]),
                     r=['xt'], w=['junk', ('ss', j)])
            P.op('dve', lambda e: e.tensor_scalar(out=ms[:, 0:nblk], in0=ss[:, 0:nblk], scalar1=1.0 / 1024, scalar2=EPS, op0=ALU.mult, op1=ALU.add),
                 r=[('ss', j) for j in range(nblk)], w=['ms'])
            P.op('act', lambda e: e.activation(out=sq[:, 0:nblk], in_=ms[:, 0:nblk], func=AF.Sqrt), r=['ms'], w=['sq'])
            P.op('dve', lambda e: e.reciprocal(out=rstd[:, 0:nblk], in_=sq[:, 0:nblk]), r=['sq'], w=['rstd'])
            for j in range(nblk):
                P.op('act', lambda e, j=j: e.activation(out=xs[:, j, :], in_=xt[:, j, :], func=AF.Copy, scale=rstd[:, j:j + 1]),
                     r=['xt', 'rstd'], w=[('xs', j)])
            if next_rows is not None:
                P.dma('act', xt[:, 0:nblk, :], next_rows.rearrange("(j p) d -> p j d", p=128), w=['xt'])
            for dc in range(8):
                half = dc % 2
                for j in range(nblk):
                    P.op('pe', lambda e, j=j, dc=dc, half=half: e.transpose(PTs[half][:, j * 128:(j + 1) * 128],
                                                                            xs[:, j, dc * 128:(dc + 1) * 128], identb[:]),
                         r=[('xs', j), 'identb'], w=[('pt', half)])
                P.op('dve', lambda e, dc=dc, half=half: e.tensor_scalar(out=xnT[:, dc, 0:nblk * 128], in0=PTs[half][:, 0:nblk * 128],
                                                                         scalar1=vec[:, gcol + dc:gcol + dc + 1], scalar2=None, op0=ALU.mult),
                     r=[('pt', half)], w=[('xnT', dc)])

        def alloc_front(ph, nblk):
            S = {}
            S['xt'] = ph.enter_context(_sbuf("xt", [128, nblk, 1024], F32))
            S['xs'] = ph.enter_context(_sbuf("xs", [128, nblk, 1024], BF16))
            S['xnT'] = ph.enter_context(_sbuf("xnT", [128, 8, nblk * 128], BF16))
            S['junk'] = ph.enter_context(_sbuf("junk", [128, 1024], BF16))
            for nm in ('ss', 'ms', 'sq', 'rstd'):
                S[nm] = ph.enter_context(_sbuf(nm, [128, 4], F32))
            return S

        def load_w_bf16(dst, src, row0, nrows_chunks, col0, ncols, key):
            for c in range(nrows_chunks):
                for s0 in range(0, ncols, 1024):
                    n = min(1024, ncols - s0)
                    P.dma('pool', dst[:, c, s0:s0 + n], src[row0 + c * 128: row0 + (c + 1) * 128, col0 + s0: col0 + s0 + n], w=[(key, c)])

        def finish_early():
            with ExitStack() as phx:
                tt = phx.enter_context(_sbuf("early", [128, 1024], F32))
                P.dma('sp', tt[:], xo[0:128, :], w=['early'])
                P.dma('sp', out_d[0:128, :], tt[:], r=['early'], w=['early_o'])
                P.barrier()
                P.emit()
            return nc

        with ExitStack() as ph:
            sb = lambda name, shape, dt=F32: ph.enter_context(_sbuf(name, shape, dt))
            S = alloc_front(ph, 4)
            wkvu = sb("wkvu", [128, 8, 2048], BF16)
            wa = sb("wa", [128, 8, 128], BF16); wx = sb("wx", [128, 8, 128], BF16)
            selt = sb("selt", [128, 8])
            uext = sb("uext", [128, 8, 515]); hT = sb("hT", [128, 8, 512]); hlast = sb("hlast", [128, 8])
            uc = [sb("uc%d" % i, [128, 512]) for i in range(8)]
            ucb = [sb("ucb%d" % i, [128, 512], BF16) for i in range(2)]
            rr = [sb("rr%d" % i, [128, 512]) for i in range(8)]
            ii = [sb("ii%d" % i, [128, 512]) for i in range(8)]
            aa = [sb("aa%d" % i, [128, 512]) for i in range(8)]
            kst = [sb("kst%d" % i, [128, 512], BF16) for i in range(2)]
            vst = [sb("vst%d" % i, [128, 512], BF16) for i in range(2)]
            hst = sb("hst", [128, 8, 128], BF16)
            load_w_bf16(wkvu, w_in, 0, 8, 512, 2048, 'wkvu')
            P.dma('pool', wa[:], lru_wa.rearrange("n c d -> c n d"), w=['wa'])
            P.dma('pool', wx[:], lru_wx.rearrange("n c d -> c n d"), w=['wx'])
            P.dma('sp', selt[:], sel_d[:, :], w=['selt'])
            P.op('pool', lambda e: e.memset(uext[:, :, 0:3], 0.0), w=[('halo', c_) for c_ in range(8)])
            for tc in range(16):
                front_end(S, xb[tc * 512:(tc + 1) * 512, :], V_GMIX, 4, 'L', next_rows=(xb[(tc + 1) * 512:(tc + 2) * 512, :] if tc < 15 else None), first=(tc == 0))
                xnT = S['xnT']
                xk = [('xnT', dc) for dc in range(8)]
                def kgroup(pair):
                    pb = PB[2 + pair % 2]
                    for dc in range(8):
                        P.op('pe', lambda e, dc=dc: e.matmul(pb[:], lhsT=wkvu[:, dc, pair * 128:(pair + 1) * 128], rhs=xnT[:, dc, :],
                                                             start=(dc == 0), stop=(dc == 7)),
                             r=[('wkvu', dc), ('xnT', dc)], w=[('pb', 2 + pair % 2)])
                    P.op('act', lambda e: e.copy(out=kst[pair % 2][:], in_=pb[:]), r=[('pb', 2 + pair % 2)], w=[('kst', pair % 2)])
                    P.dma('sp', kT_d[pair, :, tc * 512:(tc + 1) * 512], kst[pair % 2][:], r=[('kst', pair % 2)], w=[('kT_d', pair, tc)])

                def vgroup(j):
                    pb = PB[4 + j % 2]
                    for dc in range(8):
                        P.op('pe', lambda e, dc=dc: e.matmul(pb[:], lhsT=xnT[:, dc, j * 128:(j + 1) * 128], rhs=wkvu[:, dc, 512:1024],
                                                             start=(dc == 0), stop=(dc == 7)),
                             r=[('wkvu', dc), ('xnT', dc)], w=[('pb', 4 + j % 2)])
                    P.op('dve', lambda e: e.tensor_copy(out=vst[j % 2][:], in_=pb[:]), r=[('pb', 4 + j % 2)], w=[('vst', j % 2)])
                    P.dma('sp', v_d[tc * 4 + j, :, :], vst[j % 2][:], r=[('vst', j % 2)], w=[('v_d', tc * 4 + j)])

                for cc in range(8):
                    pb = PB[cc % 2]
                    for dc in range(8):
                        P.op('pe', lambda e, pb=pb, dc=dc, cc=cc: e.matmul(pb[:], lhsT=wkvu[:, dc, 1024 + cc * 128:1024 + (cc + 1) * 128], rhs=xnT[:, dc, :],
                                                                           start=(dc == 0), stop=(dc == 7)),
                             r=[('wkvu', dc), ('xnT', dc)], w=[('pb', cc % 2)])
                    P.op('act', lambda e, pb=pb, cc=cc: e.copy(out=uext[:, cc, 3:515], in_=pb[:]), r=[('pb', cc % 2)], w=[('uext', cc)])

                kvq = [lambda p_=p_: kgroup(p_) for p_ in range(4)] + [lambda j_=j_: vgroup(j_) for j_ in range(4)]

                def s1(cc):
                    kU = [('uext', cc), ('halo', cc)]
                    P.op('dve', lambda e: e.tensor_scalar(out=uc[cc][:], in0=uext[:, cc, 3:515], scalar1=vec[:, V_CW + 24 + cc:V_CW + 25 + cc],
                                                          scalar2=vec[:, V_CB + cc:V_CB + cc + 1], op0=ALU.mult, op1=ALU.add),
                         r=kU, w=[('uc', cc)])
                    for jj in range(3):
                        P.op('dve', lambda e, jj=jj: e.scalar_tensor_tensor(out=uc[cc][:], in0=uext[:, cc, jj:jj + 512],
                                                                            scalar=vec[:, V_CW + jj * 8 + cc:V_CW + jj * 8 + cc + 1],
                                                                            in1=uc[cc][:], op0=ALU.mult, op1=ALU.add),
                             r=kU + [('uc', cc)], w=[('uc', cc)])
                    P.op('pool', lambda e: e.tensor_copy(out=uext[:, cc, 0:3], in_=uext[:, cc, 512:515]), r=kU, w=kU)
                    b = cc % 2
                    P.op('act', lambda e: e.copy(out=ucb[b][:], in_=uc[cc][:]), r=[('uc', cc)], w=[('ucb', b)])
                    P.op('pe', lambda e: e.matmul(PB[0][:], lhsT=wa[:, cc, :], rhs=ucb[b][:], start=True, stop=True), r=['wa', ('ucb', b)], w=[('pb', 0)])
                    P.op('act', lambda e: e.activation(out=rr[cc][:], in_=PB[0][:], func=AF.Sigmoid, bias=vec[:, V_BA + cc:V_BA + cc + 1]),
                         r=[('pb', 0)], w=[('rr', cc)])
                    P.op('pe', lambda e: e.matmul(PB[1][:], lhsT=wx[:, cc, :], rhs=ucb[b][:], start=True, stop=True), r=['wx', ('ucb', b)], w=[('pb', 1)])
                    P.op('act', lambda e: e.activation(out=ii[cc][:], in_=PB[1][:], func=AF.Sigmoid, bias=vec[:, V_BX + cc:V_BX + cc + 1]),
                         r=[('pb', 1)], w=[('ii', cc)])

                def s2(cc):
                    P.op('act', lambda e: e.activation(out=aa[cc][:], in_=rr[cc][:], func=AF.Exp, scale=cL[:, cc:cc + 1]), r=[('rr', cc), 'cL'], w=[('aa', cc)])
                    P.op('pool', lambda e: e.tensor_tensor(out=rr[cc][:], in0=aa[cc][:], in1=aa[cc][:], op=ALU.mult), r=[('aa', cc)], w=[('rr', cc)])
                    P.op('pool', lambda e: e.tensor_tensor(out=ii[cc][:], in0=ii[cc][:], in1=uc[cc][:], op=ALU.mult), r=[('ii', cc), ('uc', cc)], w=[('ii', cc)])

                def s3(cc):
                    P.op('act', lambda e: e.activation(out=rr[cc][:], in_=rr[cc][:], func=AF.Sqrt, scale=-1.0, bias=1.0), r=[('rr', cc)], w=[('rr', cc)])
                    P.op('pool', lambda e: e.tensor_tensor(out=ii[cc][:], in0=ii[cc][:], in1=rr[cc][:], op=ALU.mult), r=[('ii', cc), ('rr', cc)], w=[('ii', cc)])

                def s4(cc):
                    if tc == 0:
                        P.op('dve', lambda e: e.tensor_tensor_scan(out=hT[:, cc, :], data0=aa[cc][:], data1=ii[cc][:], initial=0.0, op0=ALU.mult, op1=ALU.add),
                             r=[('aa', cc), ('ii', cc)], w=[('hT', cc)])
                    else:
                        P.op('dve', lambda e: e.tensor_tensor_scan(out=hT[:, cc, :], data0=aa[cc][:], data1=ii[cc][:], initial=hlast[:, cc:cc + 1],
                                                                   op0=ALU.mult, op1=ALU.add),
                             r=[('aa', cc), ('ii', cc), ('hlast', cc)], w=[('hT', cc)])
                    P.op('dve', lambda e: e.tensor_copy(out=hlast[:, cc:cc + 1], in_=hT[:, cc, 511:512]), r=[('hT', cc)], w=[('hlast', cc)])
                    so = 4 * (tc % 2)
                    P.op('dve', lambda e: e.tensor_scalar(out=hst[:, cc, :], in0=hT[:, cc, 0:128], scalar1=selt[:, so:so + 1], scalar2=None, op0=ALU.mult),
                         r=[('hT', cc), 'selt'], w=[('hst', cc)])
                    for pp in range(1, 4):
                        P.op('dve', lambda e, pp=pp: e.scalar_tensor_tensor(out=hst[:, cc, :], in0=hT[:, cc, pp * 128:(pp + 1) * 128],
                                                                            scalar=selt[:, so + pp:so + pp + 1], in1=hst[:, cc, :],
                                                                            op0=ALU.mult, op1=ALU.add),
                             r=[('hT', cc), ('hst', cc)], w=[('hst', cc)])

                h0 = range(0, 4); h1 = range(4, 8)
                for cc in h0:
                    s1(cc); kvq[cc]()
                for cc in h0:
                    s2(cc)
                for cc in h1:
                    s1(cc)
                for cc in h1:
                    kvq[cc]()
                for cc in h0:
                    s3(cc)
                for cc in h0:
                    s4(cc)
                for cc in h1:
                    s2(cc)
                for cc in h1:
                    s3(cc)
                for cc in h1:
                    s4(cc)
                P.dma('sp', hown_d[:, :, tc * 128:(tc + 1) * 128], hst[:], r=[('hst', cc) for cc in range(8)], w=[('hown_d', tc)])
            P.barrier()
            P.emit()

        if STOP == 1:
            return finish_early()
        with ExitStack() as ph:
            sb = lambda name, shape, dt=F32: ph.enter_context(_sbuf(name, shape, dt))
            S = alloc_front(ph, 4)
            wqg = sb("wqg", [128, 8, 1536], BF16); wm = sb("wm", [128, 8, 2048], BF16)
            hoc = sb("hoc", [128, 8, 512], BF16); hg = sb("hg", [128, 8, 512], BF16); gm = sb("gm", [128, 16, 512], BF16)
            qst = [sb("qst%d" % i, [128, 512], BF16) for i in range(2)]
            gl = [sb("gl%d" % i, [128, 512]) for i in range(2)]
            load_w_bf16(wqg[:, :, 0:512], w_in, 0, 8, 0, 512, 'wq')
            load_w_bf16(wqg[:, :, 512:1536], w_in, 0, 8, 2560, 1024, 'wg')
            load_w_bf16(wm, w_merge, 0, 8, 0, 2048, 'wm')
            for oc in range(4):
                front_end(S, xo[oc * 512:(oc + 1) * 512, :], V_GMIX, 4, 'O', next_rows=(xo[(oc + 1) * 512:(oc + 2) * 512, :] if oc < 3 else None), first=(oc == 0))
                xnT = S['xnT']
                P.dma('sp', hoc[:], hown_d[:, :, oc * 512:(oc + 1) * 512], w=['hoc'])
                for pair in range(4):
                    pb = PB[2 + pair % 2]
                    for dc in range(8):
                        P.op('pe', lambda e, pb=pb, dc=dc, pair=pair: e.matmul(pb[:], lhsT=wqg[:, dc, pair * 128:(pair + 1) * 128], rhs=xnT[:, dc, :],
                                                                               start=(dc == 0), stop=(dc == 7)),
                             r=[('wq', dc), ('xnT', dc)], w=[('pb', 2 + pair % 2)])
                    P.op('act', lambda e, pb=pb, pair=pair: e.activation(out=qst[pair % 2][:], in_=pb[:], func=AF.Copy, scale=0.125),
                         r=[('pb', 2 + pair % 2)], w=[('qst', pair % 2)])
                    P.dma('sp', qT_d[pair, :, oc * 512:(oc + 1) * 512], qst[pair % 2][:], r=[('qst', pair % 2)], w=[('qT_d', pair, oc)])
                for cc in range(8):
                    pb = PB[cc % 2]
                    for dc in range(8):
                        P.op('pe', lambda e, pb=pb, dc=dc, cc=cc: e.matmul(pb[:], lhsT=wqg[:, dc, 512 + cc * 128:512 + (cc + 1) * 128], rhs=xnT[:, dc, :],
                                                                           start=(dc == 0), stop=(dc == 7)),
                             r=[('wg', dc), ('xnT', dc)], w=[('pb', cc % 2)])
                    P.op('act', lambda e, pb=pb, cc=cc: e.activation(out=gl[cc % 2][:], in_=pb[:], func=AF.Gelu), r=[('pb', cc % 2)], w=[('gl', cc % 2)])
                    P.op('dve', lambda e, cc=cc: e.tensor_tensor(out=hg[:, cc, :], in0=gl[cc % 2][:], in1=hoc[:, cc, :], op=ALU.mult),
                         r=[('gl', cc % 2), 'hoc'], w=['hg'])
                P.dma('sp', hg_d[:, :, oc * 512:(oc + 1) * 512], hg[:], r=['hg'], w=[('hg_d', oc)])
                for c in range(16):
                    pb = PB[4 + c % 2]
                    for dc in range(8):
                        P.op('pe', lambda e, pb=pb, dc=dc, c=c: e.matmul(pb[:], lhsT=wm[:, dc, c * 128:(c + 1) * 128], rhs=xnT[:, dc, :],
                                                                         start=(dc == 0), stop=(dc == 7)),
                             r=[('wm', dc), ('xnT', dc)], w=[('pb', 4 + c % 2)])
                    P.op('act', lambda e, pb=pb, c=c: e.activation(out=gm[:, c, :], in_=pb[:], func=AF.Sigmoid, bias=vec[:, V_BM + c:V_BM + c + 1]),
                         r=[('pb', 4 + c % 2)], w=['gm'])
                P.dma('sp', gm_d[:, :, oc * 512:(oc + 1) * 512], gm[:], r=['gm'], w=[('gm_d', oc)])
            P.barrier()
            P.emit()

        if STOP == 2:
            return finish_early()
        with ExitStack() as ph:
            sb = lambda name, shape, dt=F32: ph.enter_context(_sbuf(name, shape, dt))
            kT = sb("kT", [128, 4, 8192], BF16); vv = sb("vv", [128, 64, 512], BF16); qT = sb("qTp", [128, 8, 2048], BF16)
            maskt = sb("maskt", [128, 1024]); ntri = sb("ntri", [128, 128]); ntrib = sb("ntrib", [128, 128], BF16)
            ones2 = sb("ones2", [128, 2]); ones2b = sb("ones2b", [128, 2], BF16)
            e_t = [sb("e_t%d" % i, [128, 512]) for i in range(2)]
            sp_t = [sb("sp_t%d" % i, [128, 512]) for i in range(2)]
            sphi = [sb("sphi%d" % i, [128, 512], BF16) for i in range(4)]
            splo = [sb("splo%d" % i, [128, 512], BF16) for i in range(4)]
            w_t = [sb("w_t%d" % i, [128, 512], BF16) for i in range(2)]
            Xc = [sb("Xc%d" % i, [128, 4]) for i in range(3)]
            Rb = [sb("Rb%d" % i, [128, 1]) for i in range(4)]
            Eself = sb("Eself", [128, 4, 6]); Esel = sb("Esel", [128, 4, 6], BF16)
            f_t = [sb("f_t%d" % i, [128, 4]) for i in range(3)]
            oacc = [sb("oacc%d" % i, [128, 64]) for i in range(2)]
            att = [sb("att%d" % i, [128, 512], BF16) for i in range(2)]
            attT = [sb("attT%d" % i, [128, 4, 128], BF16) for i in range(2)]
            P.op('pool', lambda e: e.memset(qT[:], 0.0), w=[('qT', h_) for h_ in range(8)])
            for pair in range(4):
                for hh in range(2):
                    P.dma('sp', kT[:, pair, hh * 4096:(hh + 1) * 4096], kT_d[pair, :, hh * 4096:(hh + 1) * 4096], w=[('kT', pair, hh)])
                for half in range(2):
                    P.dma('sp', qT[half * 64:(half + 1) * 64, 2 * pair + half, :], qT_d[pair, half * 64:(half + 1) * 64, :], w=[('qT', 2 * pair + half)])
            for b8 in range(8):
                P.dma('sp', vv[:, b8 * 8:(b8 + 1) * 8, :], v_d[b8 * 8:(b8 + 1) * 8, :, :].rearrange("b p c -> p b c"), w=[('vv', b8)])
            P.dma('sp', maskt[:], masks_d[:, :], w=['maskt'])
            P.dma('sp', ntri[:], ntri_d[:, :], w=['ntri'])
            P.op('dve', lambda e: e.tensor_copy(out=ntrib[:], in_=ntri[:]), r=['ntri'], w=['ntrib'])
            P.op('pool', lambda e: e.memset(ones2[:], 1.0), w=['ones2'])
            P.op('dve', lambda e: e.tensor_copy(out=ones2b[:], in_=ones2[:]), r=['ones2'], w=['ones2b'])
            P.op('pool', lambda e: e.memset(Eself[:], 0.0), w=['Eself'])
            for s_ in range(4):
                if s_ >= 1:
                    P.op('pool', lambda e, s_=s_: e.memset(Eself[:, s_, 0:s_], 1.0), r=['Eself'], w=['Eself'])
                P.op('pool', lambda e, s_=s_: e.memset(Eself[:, s_, 4:5], 1.0), r=['Eself'], w=['Eself'])
            P.op('dve', lambda e: e.tensor_copy(out=Esel[:], in_=Eself[:]), r=['Eself'], w=['Esel'])

            groups = []
            for slot in range(16):
                ng = (slot // 2) * 2 + 1 + (slot % 2)
                for h in range(8):
                    for g in range(ng):
                        groups.append((slot, h, g, ng))
            NG = len(groups)

            def zmm(gi, bank, closed):
                slot, h, g, ng = groups[gi]
                pair = h // 2
                base = 4 * ng - 4 * (g + 1)
                for s in range(4):
                    kb = base + s
                    st = True if closed else (s == 0)
                    sp_ = True if closed else False
                    P.op('pe', lambda e, s=s, kb=kb, pair=pair, h=h, slot=slot, st=st, sp_=sp_: e.matmul(
                        PB[bank][:, s * 128:(s + 1) * 128], lhsT=kT[:, pair, kb * 128:(kb + 1) * 128],
                        rhs=qT[:, h, slot * 128:(slot + 1) * 128], start=st, stop=sp_),
                        r=[('kT', pair, kb // 32), ('qT', h)], w=[('pb', bank)])

            def stageA(gi):
                slot, h, g, ng = groups[gi]
                za = gi % 2
                zmm(gi, za, True)
                P.op('act', lambda e: e.activation(out=e_t[gi % 2][:], in_=PB[za][:], func=AF.Exp), r=[('pb', za)], w=[('e_t', gi % 2)])
                P.op('act', lambda e: e.activation(out=sp_t[gi % 2][:], in_=e_t[gi % 2][:], func=AF.Ln, bias=1.0), r=[('e_t', gi % 2)], w=[('sp_t', gi % 2)])
                if g == 0:
                    mo = 512 * (slot % 2)
                    P.op('dve', lambda e, mo=mo: e.tensor_tensor(out=sp_t[gi % 2][:], in0=sp_t[gi % 2][:], in1=maskt[:, mo:mo + 512], op=ALU.mult),
                         r=[('sp_t', gi % 2), 'maskt'], w=[('sp_t', gi % 2)])
                P.op('dve', lambda e: e.tensor_copy(out=sphi[gi % 4][:], in_=sp_t[gi % 2][:]), r=[('sp_t', gi % 2)], w=[('sphi', gi % 4)])
                P.op('pool', lambda e: e.tensor_tensor(out=splo[gi % 4][:], in0=sp_t[gi % 2][:], in1=sphi[gi % 4][:], op=ALU.subtract),
                     r=[('sp_t', gi % 2), ('sphi', gi % 4)], w=[('splo', gi % 4)])

            def stageB(gi):
                slot, h, g, ng = groups[gi]
                ab = 2 + gi % 2
                zmm(gi, ab, False)
                P.op('pe', lambda e: e.matmul(PB[ab][:], lhsT=ntrib[:], rhs=sphi[gi % 4][:], start=False, stop=False),
                     r=[('sphi', gi % 4), 'ntrib'], w=[('pb', ab)])
                P.op('pe', lambda e: e.matmul(PB[ab][:], lhsT=ntrib[:], rhs=splo[gi % 4][:], start=False, stop=True),
                     r=[('splo', gi % 4), 'ntrib'], w=[('pb', ab)])
                P.op('act', lambda e: e.activation(out=w_t[gi % 2][:], in_=PB[ab][:], func=AF.Exp), r=[('pb', ab)], w=[('w_t', gi % 2)])
                for s in range(4):
                    P.op('pe', lambda e, s=s: e.matmul(PB[4][:, 0:6], lhsT=sphi[gi % 4][:, s * 128:(s + 1) * 128], rhs=Esel[:, s, :], start=(s == 0), stop=False),
                         r=[('sphi', gi % 4), 'Esel'], w=[('pb', 4)])
                    P.op('pe', lambda e, s=s: e.matmul(PB[4][:, 0:6], lhsT=splo[gi % 4][:, s * 128:(s + 1) * 128], rhs=Esel[:, s, :], start=False, stop=(s == 3)),
                         r=[('splo', gi % 4), 'Esel'], w=[('pb', 4)])
                if g == 0:
                    mo = 512 * (slot % 2)
                    P.op('dve', lambda e, mo=mo: e.tensor_tensor(out=w_t[gi % 2][:], in0=w_t[gi % 2][:], in1=maskt[:, mo:mo + 512], op=ALU.mult),
                         r=[('w_t', gi % 2), 'maskt'], w=[('w_t', gi % 2)])
                R = Rb[gi % 4]; Rn = Rb[(gi + 1) % 4]
                X = Xc[gi % 3]
                if g == 0:
                    P.op('pool', lambda e, R=R: e.memset(R[:], 0.0), w=[('R', gi % 4)])
                P.op('dve', lambda e, R=R, X=X: e.tensor_scalar(out=X[:], in0=PB[4][:, 0:4], scalar1=R[:, 0:1], scalar2=None, op0=ALU.add),
                     r=[('pb', 4), ('R', gi % 4)], w=[('X', gi % 3)])
                if g != ng - 1:
                    P.op('dve', lambda e, R=R, Rn=Rn: e.tensor_scalar(out=Rn[:], in0=PB[4][:, 4:5], scalar1=R[:, 0:1], scalar2=None, op0=ALU.add),
                         r=[('pb', 4), ('R', gi % 4)], w=[('R', (gi + 1) % 4)])
                P.op('act', lambda e, X=X: e.activation(out=f_t[gi % 3][:], in_=X[:], func=AF.Exp, scale=-1.0),
                     r=[('X', gi % 3)], w=[('f_t', gi % 3)])

            def stageC(gi):
                slot, h, g, ng = groups[gi]
                base = 4 * ng - 4 * (g + 1)
                hidx = slot * 8 + h
                oa = oacc[hidx % 2]
                ok = ('oacc', hidx % 2)
                for s in range(4):
                    kb = base + s
                    P.op('pe', lambda e, s=s, kb=kb, h=h: e.matmul(PB[5][:, s * 64:(s + 1) * 64], lhsT=w_t[gi % 2][:, s * 128:(s + 1) * 128],
                                                                   rhs=vv[:, kb, h * 64:(h + 1) * 64], start=True, stop=True),
                         r=[('w_t', gi % 2), ('vv', kb // 8)], w=[('pb', 5)])
                for s in range(4):
                    if g == 0 and s == 0:
                        P.op('dve', lambda e, s=s, oa=oa: e.tensor_scalar(out=oa[:], in0=PB[5][:, s * 64:(s + 1) * 64], scalar1=f_t[gi % 3][:, s:s + 1], scalar2=None, op0=ALU.mult),
                             r=[('pb', 5), ('f_t', gi % 3)], w=[ok])
                    else:
                        P.op('dve', lambda e, s=s, oa=oa: e.scalar_tensor_tensor(out=oa[:], in0=PB[5][:, s * 64:(s + 1) * 64], scalar=f_t[gi % 3][:, s:s + 1],
                                                                                 in1=oa[:], op0=ALU.mult, op1=ALU.add),
                             r=[('pb', 5), ('f_t', gi % 3), ok], w=[ok])
                if g == ng - 1:
                    P.op('pool', lambda e, oa=oa, h=h, slot=slot: e.tensor_copy(out=att[slot % 2][:, h * 64:(h + 1) * 64], in_=oa[:]),
                         r=[ok], w=[('att', slot % 2, h)])
                    if h == 7:
                        for c4 in range(4):
                            P.op('pe', lambda e, c4=c4, slot=slot: e.transpose(PT[:, c4 * 128:(c4 + 1) * 128], att[slot % 2][:, c4 * 128:(c4 + 1) * 128], identb[:]),
                                 r=[('att', slot % 2, 2 * c4), ('att', slot % 2, 2 * c4 + 1), 'identb'], w=['ptA'])
                        P.op('dve', lambda e, slot=slot: e.tensor_copy(out=attT[slot % 2][:], in_=PT[:, 0:512].rearrange("p (c q) -> p c q", c=4)),
                             r=['ptA'], w=[('attT', slot % 2)])
                        P.dma('sp', attT_d[:, :, slot * 128:(slot + 1) * 128], attT[slot % 2][:], r=[('attT', slot % 2)], w=[('attT_d', slot)])

            for it in range(NG + 3):
                if it < NG:
                    stageA(it)
                if 0 <= it - 2 < NG:
                    stageB(it - 2)
                if 0 <= it - 3 < NG:
                    stageC(it - 3)
            P.barrier()
            P.emit()

        if STOP == 3:
            return finish_early()
        with ExitStack() as ph:
            sb = lambda name, shape, dt=F32: ph.enter_context(_sbuf(name, shape, dt))
            wao = sb("wao", [128, 4, 1024], BF16); wlo = sb("wlo", [128, 8, 1024], BF16); wo = sb("wo", [128, 8, 1024], BF16)
            attc = sb("attc", [128, 4, 512], BF16); hgc = sb("hgc", [128, 8, 512], BF16); gmc = sb("gmc", [128, 16, 512], BF16)
            t1 = [sb("t1%d" % i, [128, 512]) for i in range(2)]
            t2 = [sb("t2%d" % i, [128, 512]) for i in range(2)]
            mT = sb("mT", [128, 8, 512], BF16)
            xres = sb("xres", [128, 4, 1024]); hres = sb("hres", [128, 4, 1024])
            load_w_bf16(wao, w_att_out, 0, 4, 0, 1024, 'wao')
            load_w_bf16(wlo, w_lru_out, 0, 8, 0, 1024, 'wlo')
            load_w_bf16(wo, w_out, 0, 8, 0, 1024, 'wo')
            for oc in range(4):
                sl = slice(oc * 512, (oc + 1) * 512)
                P.dma('sp', attc[:], attT_d[:, :, sl], w=['attc'])
                P.dma('sp', hgc[:], hg_d[:, :, sl], w=['hgc'])
                P.dma('sp', gmc[:], gm_d[:, :, sl], w=['gmc'])
                P.dma('sp', xres[:], xo[sl, :].rearrange("(j p) d -> p j d", p=128), w=['xres'])
                for dmc in range(8):
                    pa = PB[(2 * dmc) % 4]; pl = PB[(2 * dmc + 1) % 4]
                    ka = ('pb', (2 * dmc) % 4); kl = ('pb', (2 * dmc + 1) % 4)
                    for cc in range(4):
                        P.op('pe', lambda e, pa=pa, cc=cc, dmc=dmc: e.matmul(pa[:], lhsT=wao[:, cc, dmc * 128:(dmc + 1) * 128], rhs=attc[:, cc, :],
                                                                             start=(cc == 0), stop=(cc == 3)), r=[('wao', cc), 'attc'], w=[ka])
                    for cc in range(8):
                        P.op('pe', lambda e, pl=pl, cc=cc, dmc=dmc: e.matmul(pl[:], lhsT=wlo[:, cc, dmc * 128:(dmc + 1) * 128], rhs=hgc[:, cc, :],
                                                                             start=(cc == 0), stop=(cc == 7)), r=[('wlo', cc), 'hgc'], w=[kl])
                    b = dmc % 2
                    P.op('dve', lambda e, pa=pa, dmc=dmc, b=b: e.tensor_tensor(out=t1[b][:], in0=pa[:], in1=gmc[:, dmc, :], op=ALU.mult), r=[ka, 'gmc'], w=[('t1', b)])
                    P.op('dve', lambda e, pl=pl, dmc=dmc, b=b: e.tensor_tensor(out=t2[b][:], in0=pl[:], in1=gmc[:, 8 + dmc, :], op=ALU.mult), r=[kl, 'gmc'], w=[('t2', b)])
                    P.op('pool', lambda e, dmc=dmc, b=b: e.tensor_tensor(out=mT[:, dmc, :], in0=t1[b][:], in1=t2[b][:], op=ALU.add),
                         r=[('t1', b), ('t2', b)], w=[('mT', dmc)])
                for j in range(4):
                    for n in range(2):
                        pi = 4 + (j * 2 + n) % 2
                        pb = PB[pi]
                        for dmc in range(8):
                            P.op('pe', lambda e, pb=pb, dmc=dmc, j=j, n=n: e.matmul(pb[:], lhsT=mT[:, dmc, j * 128:(j + 1) * 128], rhs=wo[:, dmc, n * 512:(n + 1) * 512],
                                                                                    start=(dmc == 0), stop=(dmc == 7)), r=[('mT', dmc), ('wo', dmc)], w=[('pb', pi)])
                        P.op('dve', lambda e, pb=pb, j=j, n=n: e.tensor_tensor(out=hres[:, j, n * 512:(n + 1) * 512], in0=pb[:], in1=xres[:, j, n * 512:(n + 1) * 512], op=ALU.add),
                             r=[('pb', pi), 'xres'], w=['hres'])
                P.dma('sp', h_d[sl, :].rearrange("(j p) d -> p j d", p=128), hres[:], r=['hres'], w=[('h_d', oc)])
            P.barrier()
            P.emit()

        if STOP == 4:
            return finish_early()
        with ExitStack() as ph:
            sb = lambda name, shape, dt=F32: ph.enter_context(_sbuf(name, shape, dt))
            phH = ExitStack()
            sbH = lambda name, shape, dt=F32: phH.enter_context(_sbuf(name, shape, dt))
            H = sbH("H", [128, 16, 1024])
            junkE = sbH("junkE", [128, 1024], BF16); ones1 = sbH("ones1", [1, 128])
            sm = {nm: sbH("e_" + nm, [128, 8]) for nm in ('ss', 'ms', 'sq', 'rstd', 'mx', 'negm', 'sum', 'rs')}
            xn2T = sb("xn2T", [128, 8, 2048], BF16)
            G = sb("G", [128, 16, 32])
            xsf = sb("xsf", [128, 1024]); xTf = sb("xTf", [128, 8, 128])
            wr = sb("wr", [128, 8, 32]); brt = sb("brt", [1, 32])
            lg = sb("lg", [128, 32]); mk = sb("mk", [128, 32]); ex = sb("ex", [128, 32])
            wgt = [sb("wgt%d" % i, [128, 8, 1024], BF16) for i in range(2)]
            wdt = [sb("wdt%d" % i, [128, 4, 1024], BF16) for i in range(2)]
            bdall = sb("bdall", [32, 1024]); GT = sb("GT", [32, 128])
            gc = [sb("gc%d" % i, [128, 512]) for i in range(2)]
            sg = [sb("sg%d" % i, [128, 512]) for i in range(2)]
            u1 = [sb("u1%d" % i, [128, 512]) for i in range(2)]
            actT = [sb("actT%d" % i, [128, 4, 512], BF16) for i in range(2)]
            for oc in range(4):
                P.dma('sp', H[:, oc * 4:(oc + 1) * 4, :], h_d[oc * 512:(oc + 1) * 512, :].rearrange("(j p) d -> p j d", p=128), w=[('H', oc * 4 + j) for j in range(4)])
            P.dma('sp', wr[:], w_router.rearrange("(c p) e -> p c e", p=128), w=['wr'])
            P.dma('sp', brt[:], b_router[:, :], w=['brt'])
            P.dma('sp', bdall[:], b_dn[:, :], w=['bdall'])
            P.op('pool', lambda e: e.memset(ones1[:], 1.0), w=['ones1'])
            for blk in range(16):
                P.op('act', lambda e, blk=blk: e.activation(out=junkE[:], in_=H[:, blk, :], func=AF.Square, accum_out=sm['ss'][:, 0:1]),
                     r=[('H', blk)], w=['junkE', 'ss'])
                P.op('dve', lambda e: e.tensor_scalar(out=sm['ms'][:, 0:1], in0=sm['ss'][:, 0:1], scalar1=1.0 / 1024, scalar2=EPS, op0=ALU.mult, op1=ALU.add), r=['ss'], w=['ms'])
                P.op('act', lambda e: e.activation(out=sm['sq'][:, 0:1], in_=sm['ms'][:, 0:1], func=AF.Sqrt), r=['ms'], w=['sq'])
                P.op('dve', lambda e: e.reciprocal(out=sm['rstd'][:, 0:1], in_=sm['sq'][:, 0:1]), r=['sq'], w=['rstd'])
                P.op('act', lambda e, blk=blk: e.activation(out=xsf[:], in_=H[:, blk, :], func=AF.Copy, scale=sm['rstd'][:, 0:1]), r=[('H', blk), 'rstd'], w=['xsf'])
                for dc in range(8):
                    pi = dc % 2
                    P.op('pe', lambda e, dc=dc, pi=pi: e.transpose(PB[pi][:, 0:128], xsf[:, dc * 128:(dc + 1) * 128], identf[:]), r=['xsf', 'identf'], w=[('pb', pi)])
                    P.op('dve', lambda e, dc=dc, pi=pi: e.tensor_scalar(out=xTf[:, dc, :], in0=PB[pi][:, 0:128], scalar1=vec[:, V_GMOE + dc:V_GMOE + dc + 1], scalar2=None, op0=ALU.mult),
                         r=[('pb', pi)], w=[('xTf', dc)])
                    P.op('pool', lambda e, dc=dc, blk=blk: e.tensor_copy(out=xn2T[:, dc, blk * 128:(blk + 1) * 128], in_=xTf[:, dc, :]), r=[('xTf', dc)], w=[('xn2T', blk)])
                for dc in range(8):
                    P.op('pe', lambda e, dc=dc: e.matmul(PB[2][:, 0:32], lhsT=xTf[:, dc, :], rhs=wr[:, dc, :], start=(dc == 0), stop=False),
                         r=[('xTf', dc), 'wr'], w=[('pb', 2)])
                P.op('pe', lambda e: e.matmul(PB[2][:, 0:32], lhsT=ones1[:], rhs=brt[:], start=False, stop=True), r=['ones1', 'brt'], w=[('pb', 2)])
                P.op('dve', lambda e: e.tensor_copy(out=lg[:], in_=PB[2][:, 0:32]), r=[('pb', 2)], w=['lg'])
                P.op('dve', lambda e: e.max(out=sm['mx'][:], in_=lg[:]), r=['lg'], w=['mx'])
                P.op('dve', lambda e: e.tensor_scalar(out=mk[:], in0=lg[:], scalar1=sm['mx'][:, 3:4], scalar2=None, op0=ALU.is_ge), r=['lg', 'mx'], w=['mk'])
                P.op('dve', lambda e: e.tensor_scalar(out=sm['negm'][:, 0:1], in0=sm['mx'][:, 0:1], scalar1=-1.0, scalar2=None, op0=ALU.mult), r=['mx'], w=['negm'])
                P.op('act', lambda e: e.activation(out=ex[:], in_=lg[:], func=AF.Exp, bias=sm['negm'][:, 0:1]), r=['lg', 'negm'], w=['ex'])
                P.op('dve', lambda e: e.tensor_tensor(out=ex[:], in0=ex[:], in1=mk[:], op=ALU.mult), r=['ex', 'mk'], w=['ex'])
                P.op('dve', lambda e: e.tensor_reduce(out=sm['sum'][:, 0:1], in_=ex[:], axis=AX.X, op=ALU.add), r=['ex'], w=['sum'])
                P.op('dve', lambda e: e.reciprocal(out=sm['rs'][:, 0:1], in_=sm['sum'][:, 0:1]), r=['sum'], w=['rs'])
                P.op('dve', lambda e, blk=blk: e.tensor_scalar(out=G[:, blk, :], in0=ex[:], scalar1=sm['rs'][:, 0:1], scalar2=None, op0=ALU.mult), r=['ex', 'rs'], w=[('G', blk)])
                P.op('pe', lambda e, blk=blk: e.transpose(PB[3][0:32, 0:128], G[:, blk, :], identf[:]), r=[('G', blk), 'identf'], w=[('pb', 3)])
                P.op('dve', lambda e: e.tensor_copy(out=GT[:], in_=PB[3][0:32, 0:128]), r=[('pb', 3)], w=['GT'])
                for n in range(2):
                    P.op('pe', lambda e, n=n: e.matmul(PB[4 + n][:], lhsT=GT[:], rhs=bdall[:, n * 512:(n + 1) * 512], start=True, stop=True), r=['GT', 'bdall'], w=[('pb', 4 + n)])
                    P.op('dve', lambda e, n=n, blk=blk: e.tensor_tensor(out=H[:, blk, n * 512:(n + 1) * 512], in0=PB[4 + n][:], in1=H[:, blk, n * 512:(n + 1) * 512], op=ALU.add),
                         r=[('pb', 4 + n), ('H', blk)], w=[('H', blk)])
            def load_expert(ei, hf):
                wb = (ei * 2 + hf) % 2
                for dc in range(8):
                    P.dma('pool', wgt[wb][:, dc, :].rearrange("p (a f) -> p a f", a=2),
                          w_gu[ei, dc * 128:(dc + 1) * 128, :].rearrange("p (a f) -> p a f", a=2)[:, :, hf * 512:(hf + 1) * 512], w=[('wgt', wb, dc)])
                for fc in range(4):
                    P.dma('pool', wdt[wb][:, fc, :], w_dn[ei, hf * 512 + fc * 128: hf * 512 + (fc + 1) * 128, :], w=[('wdt', wb, fc)])

            seq = [(ei, hf) for ei in range(32) for hf in range(2)]

            def GU(si, tg, ab):
                ei, hf = seq[si]
                wb = si % 2
                for fc in range(4):
                    pg = PB[(2 * fc) % 4]; pu = PB[(2 * fc + 1) % 4]
                    kg = ('pb', (2 * fc) % 4); ku = ('pb', (2 * fc + 1) % 4)
                    for dc in range(8):
                        P.op('pe', lambda e, pg=pg, dc=dc, fc=fc: e.matmul(pg[:], lhsT=wgt[wb][:, dc, fc * 128:(fc + 1) * 128], rhs=xn2T[:, dc, tg * 512:(tg + 1) * 512],
                                                                           start=(dc == 0), stop=(dc == 7)),
                             r=[('wgt', wb, dc)] + [('xn2T', tg * 4 + j) for j in range(4)], w=[kg])
                    for dc in range(8):
                        P.op('pe', lambda e, pu=pu, dc=dc, fc=fc: e.matmul(pu[:], lhsT=wgt[wb][:, dc, 512 + fc * 128:512 + (fc + 1) * 128], rhs=xn2T[:, dc, tg * 512:(tg + 1) * 512],
                                                                           start=(dc == 0), stop=(dc == 7)),
                             r=[('wgt', wb, dc)] + [('xn2T', tg * 4 + j) for j in range(4)], w=[ku])
                    b = fc % 2
                    cg = V_BGU + ei * 16 + hf * 4 + fc
                    cu = V_BGU + ei * 16 + 8 + hf * 4 + fc
                    P.op('dve', lambda e, pg=pg, b=b, cg=cg: e.tensor_scalar(out=gc[b][:], in0=pg[:], scalar1=vec[:, cg:cg + 1], scalar2=7.0, op0=ALU.add, op1=ALU.min), r=[kg], w=[('gc', b)])
                    P.op('act', lambda e, b=b: e.activation(out=sg[b][:], in_=gc[b][:], func=AF.Sigmoid, scale=1.702), r=[('gc', b)], w=[('sg', b)])
                    P.op('dve', lambda e, pu=pu, b=b, cu=cu: e.tensor_scalar(out=u1[b][:], in0=pu[:], scalar1=vec[:, cu:cu + 1], scalar2=-7.0, op0=ALU.add, op1=ALU.max), r=[ku], w=[('u1', b)])
                    P.op('dve', lambda e, b=b: e.tensor_scalar(out=u1[b][:], in0=u1[b][:], scalar1=7.0, scalar2=1.0, op0=ALU.min, op1=ALU.add), r=[('u1', b)], w=[('u1', b)])
                    P.op('pool', lambda e, b=b: e.tensor_tensor(out=gc[b][:], in0=gc[b][:], in1=sg[b][:], op=ALU.mult), r=[('gc', b), ('sg', b)], w=[('gc', b)])
                    P.op('pool', lambda e, b=b, fc=fc: e.tensor_tensor(out=actT[ab][:, fc, :], in0=gc[b][:], in1=u1[b][:], op=ALU.mult), r=[('gc', b), ('u1', b)], w=[('actT', ab, fc)])

            def DN(si, tg, ab):
                ei, hf = seq[si]
                wb = si % 2
                for j in range(4):
                    blk = tg * 4 + j
                    for n in range(2):
                        pi = 4 + (j * 2 + n) % 2
                        pb = PB[pi]
                        for fc in range(4):
                            P.op('pe', lambda e, pb=pb, fc=fc, j=j, n=n: e.matmul(pb[:], lhsT=actT[ab][:, fc, j * 128:(j + 1) * 128], rhs=wdt[wb][:, fc, n * 512:(n + 1) * 512],
                                                                                  start=(fc == 0), stop=(fc == 3)),
                                 r=[('actT', ab, fc), ('wdt', wb, fc)], w=[('pb', pi)])
                        P.op('dve', lambda e, pb=pb, blk=blk, n=n: e.scalar_tensor_tensor(out=H[:, blk, n * 512:(n + 1) * 512], in0=pb[:], scalar=G[:, blk, ei:ei + 1],
                                                                                          in1=H[:, blk, n * 512:(n + 1) * 512], op0=ALU.mult, op1=ALU.add),
                             r=[('pb', pi), ('G', blk), ('H', blk)], w=[('H', blk)])

            load_expert(0, 0)
            units = [(si, tg) for si in range(len(seq)) for tg in range(4)]
            for ui in range(len(units) + 1):
                if ui < len(units):
                    si, tg = units[ui]
                    GU(si, tg, ui % 2)
                if ui >= 1:
                    psi, ptg = units[ui - 1]
                    DN(psi, ptg, (ui - 1) % 2)
                if ui < len(units):
                    si, tg = units[ui]
                    if tg == 0 and si + 1 < len(seq):
                        load_expert(*seq[si + 1])
            if DBG:
                h2_d = dscr("h2_d", [2048, 1024], F32)
                for oc in range(4):
                    P.dma('sp', h2_d[oc * 512:(oc + 1) * 512, :].rearrange("(j p) d -> p j d", p=128), H[:, oc * 4:(oc + 1) * 4, :],
                          r=[('H', oc * 4 + j) for j in range(4)], w=[('h2_d', oc)])
                g_d = dscr("g_d", [128, 16, 32], F32)
                P.dma('sp', g_d[:, :, :], G[:], r=[('G', b_) for b_ in range(16)], w=['g_d'])
            P.barrier()
            P.emit()

            ph.close()
            if STOP == 5:
                phH.close()
                return finish_early()
            with ExitStack() as ph2:
                sb2 = lambda name, shape, dt=F32: ph2.enter_context(_sbuf(name, shape, dt))
                wpg = sb2("wpg", [128, 8, 1024], BF16); wpp = sb2("wpp", [128, 2, 1024], BF16)
                bpg = sb2("bpg", [1, 1024]); gpo = sb2("gpo", [128, 1024]); gfi = sb2("gfi", [128, 1024])
                hs = sb2("hs", [128, 1024], BF16); hnT = sb2("hnT", [128, 8, 128], BF16)
                pt_ = sb2("pt_", [128, 256]); ptb = sb2("ptb_", [128, 256], BF16); pT = sb2("pT", [128, 2, 128], BF16)
                gate = sb2("gate", [128, 1024]); plr = sb2("plr", [128, 1024]); ot = [sb2("ot%d" % i, [128, 1024]) for i in range(2)]
                load_w_bf16(wpg, w_plg, 0, 8, 0, 1024, 'wpg')
                load_w_bf16(wpp, w_plp, 0, 2, 0, 1024, 'wpp')
                P.dma('sp', bpg[:], b_plg[:, :], w=['bpg'])
                P.dma('sp', gpo[:], g_post.partition_broadcast(128), w=['gpo'])
                P.dma('sp', gfi[:], g_fin.partition_broadcast(128), w=['gfi'])

                def rstd_blk(src_ap, rk, col):
                    P.op('act', lambda e: e.activation(out=junkE[:], in_=src_ap, func=AF.Square, accum_out=sm['ss'][:, col:col + 1]), r=rk, w=['junkE', ('ss', col)])
                    P.op('dve', lambda e: e.tensor_scalar(out=sm['ms'][:, col:col + 1], in0=sm['ss'][:, col:col + 1], scalar1=1.0 / 1024, scalar2=EPS, op0=ALU.mult, op1=ALU.add),
                         r=[('ss', col)], w=[('ms', col)])
                    P.op('act', lambda e: e.activation(out=sm['sq'][:, col:col + 1], in_=sm['ms'][:, col:col + 1], func=AF.Sqrt), r=[('ms', col)], w=[('sq', col)])
                    P.op('dve', lambda e: e.reciprocal(out=sm['rstd'][:, col:col + 1], in_=sm['sq'][:, col:col + 1]), r=[('sq', col)], w=[('rstd', col)])

                for blk in range(16):
                    Hb = H[:, blk, :]
                    rstd_blk(Hb, [('H', blk)], 1)
                    P.op('act', lambda e, Hb=Hb: e.activation(out=hs[:], in_=Hb, func=AF.Copy, scale=sm['rstd'][:, 1:2]), r=[('H', blk), ('rstd', 1)], w=['hs'])
                    for dc in range(8):
                        P.op('pe', lambda e, dc=dc: e.transpose(PT[:, dc * 128:(dc + 1) * 128], hs[:, dc * 128:(dc + 1) * 128], identb[:]), r=['hs', 'identb'], w=['ptP'])
                    for dc in range(8):
                        P.op('dve', lambda e, dc=dc: e.tensor_scalar(out=hnT[:, dc, :], in0=PT[:, dc * 128:(dc + 1) * 128], scalar1=vec[:, V_GPL + dc:V_GPL + dc + 1], scalar2=None, op0=ALU.mult),
                             r=['ptP'], w=[('hnT', dc)])
                    for n in range(2):
                        pb = PB[n]
                        for dc in range(8):
                            P.op('pe', lambda e, pb=pb, dc=dc, n=n: e.matmul(pb[:], lhsT=hnT[:, dc, :], rhs=wpg[:, dc, n * 512:(n + 1) * 512], start=(dc == 0), stop=False),
                                 r=[('hnT', dc), ('wpg', dc)], w=[('pb', n)])
                        P.op('pe', lambda e, pb=pb, n=n: e.matmul(pb[:], lhsT=ones1[:], rhs=bpg[:, n * 512:(n + 1) * 512], start=False, stop=True), r=['ones1', 'bpg'], w=[('pb', n)])
                        P.op('act', lambda e, pb=pb, n=n: e.activation(out=gate[:, n * 512:(n + 1) * 512], in_=pb[:], func=AF.Sigmoid), r=[('pb', n)], w=['gate'])
                    P.dma('sp', pt_[:], po[blk * 128:(blk + 1) * 128, :], w=['pt_'])
                    P.op('pool', lambda e: e.tensor_copy(out=ptb[:], in_=pt_[:]), r=['pt_'], w=['ptb'])
                    for kc in range(2):
                        P.op('pe', lambda e, kc=kc: e.transpose(PTs[1][:, kc * 128:(kc + 1) * 128], ptb[:, kc * 128:(kc + 1) * 128], identb[:]), r=['ptb', 'identb'], w=['ptP1'])
                    P.op('dve', lambda e: e.tensor_copy(out=pT[:], in_=PTs[1][:, 0:256].rearrange("p (c q) -> p c q", c=2)), r=['ptP1'], w=['pT'])
                    for n in range(2):
                        pb = PB[2 + n]
                        for kc in range(2):
                            P.op('pe', lambda e, pb=pb, kc=kc, n=n: e.matmul(pb[:], lhsT=pT[:, kc, :], rhs=wpp[:, kc, n * 512:(n + 1) * 512], start=(kc == 0), stop=(kc == 1)),
                                 r=['pT', ('wpp', kc)], w=[('pb', 2 + n)])
                        P.op('act', lambda e, pb=pb, n=n: e.copy(out=plr[:, n * 512:(n + 1) * 512], in_=pb[:]), r=[('pb', 2 + n)], w=['plr'])
                    rstd_blk(plr[:], ['plr'], 2)
                    P.op('dve', lambda e: e.scalar_tensor_tensor(out=plr[:], in0=plr[:], scalar=sm['rstd'][:, 2:3], in1=gpo[:], op0=ALU.mult, op1=ALU.mult),
                         r=['plr', ('rstd', 2), 'gpo'], w=['plr'])
                    P.op('pool', lambda e: e.tensor_tensor(out=plr[:], in0=plr[:], in1=gate[:], op=ALU.mult), r=['plr', 'gate'], w=['plr'])
                    P.op('pool', lambda e, Hb=Hb: e.tensor_tensor(out=Hb, in0=Hb, in1=plr[:], op=ALU.add), r=['plr', ('H', blk)], w=[('H', blk)])
                    rstd_blk(Hb, [('H', blk)], 3)
                    o = ot[blk % 2]
                    P.op('dve', lambda e, Hb=Hb, o=o: e.scalar_tensor_tensor(out=o[:], in0=Hb, scalar=sm['rstd'][:, 3:4], in1=gfi[:], op0=ALU.mult, op1=ALU.mult),
                         r=[('H', blk), ('rstd', 3), 'gfi'], w=[('ot', blk % 2)])
                    P.dma('sp', out_d[blk * 128:(blk + 1) * 128, :], o[:], r=[('ot', blk % 2)], w=[('out', blk)])
                P.barrier()
                P.emit()
            phH.close()
    return nc


def _fm(v, n):
    return np.ascontiguousarray(np.asarray(v, np.float32).reshape(n, 128).T)


def kernel(**inp):
    f = lambda k: np.asarray(inp[k], np.float32)
    x = f("x"); p = f("p")[0]
    vecs = np.zeros((128, NV), np.float32)
    vecs[:, V_GMIX:V_GMIX + 8] = _fm(f("norm_mix_g")[0], 8)
    vecs[:, V_GMOE:V_GMOE + 8] = _fm(f("norm_moe_g")[0], 8)
    vecs[:, V_GPL:V_GPL + 8] = _fm(f("norm_pl_g")[0], 8)
    cw = f("conv_w")[0]
    for j in range(4):
        vecs[:, V_CW + j * 8:V_CW + (j + 1) * 8] = _fm(cw[j], 8)
    vecs[:, V_CB:V_CB + 8] = _fm(f("conv_b")[0], 8)
    vecs[:, V_BA:V_BA + 8] = _fm(f("lru_ba")[0], 8)
    vecs[:, V_BX:V_BX + 8] = _fm(f("lru_bx")[0], 8)
    vecs[:, V_LAM:V_LAM + 8] = _fm(f("lru_lambda")[0], 8)
    vecs[:, V_BM:V_BM + 16] = _fm(f("b_merge")[0], 16)
    bgu = f("b_gate_up")[0]
    for e in range(32):
        vecs[:, V_BGU + e * 16:V_BGU + (e + 1) * 16] = _fm(bgu[e], 16)
    ident = np.eye(128, dtype=np.float32)
    ntri = -np.tril(np.ones((128, 128), np.float32))
    tri_mask = np.triu(np.ones((128, 128), np.float32), 1)
    common = {
        "ident": ident, "ntri": ntri, "vecs": vecs,
        "w_in": f("w_in")[0], "lru_wa": f("lru_wa")[0], "lru_wx": f("lru_wx")[0],
        "w_att_out": f("w_att_out")[0], "w_lru_out": f("w_lru_out")[0], "w_merge": f("w_merge")[0], "w_out": f("w_out")[0],
        "w_router": f("w_router")[0], "b_router": f("b_router")[0].reshape(1, 32),
        "w_gate_up": f("w_gate_up")[0], "w_down": f("w_down")[0], "b_down": f("b_down")[0],
        "w_pl_gate": f("w_pl_gate")[0], "b_pl_gate": f("b_pl_gate")[0].reshape(1, 1024), "w_pl_proj": f("w_pl_proj")[0],
        "g_post": f("norm_pl_post_g")[0].reshape(1, 1024), "g_fin": f("norm_final_g").reshape(1, 1024),
    }
    in_maps = []
    own_rows = []
    for c in range(8):
        b, r = c // 4, c % 4
        blks = []
        for i in range(8):
            blks += [8 * i + r, 8 * i + 7 - r]
        rows = np.concatenate([np.arange(bk * 128, (bk + 1) * 128) for bk in blks])
        own_rows.append((b, rows))
        masks = np.zeros((128, 8, 128), np.float32)
        sel = np.zeros((128, 8), np.float32)
        for pp in range(4):
            masks[:, pp, :] = 1.0 if pp < r else (tri_mask if pp == r else 0.0)
            masks[:, 4 + pp, :] = 1.0 if pp < 3 - r else (tri_mask if pp == 3 - r else 0.0)
        sel[:, r] = 1.0
        sel[:, 4 + 3 - r] = 1.0
        m = dict(common)
        m["xb"] = np.ascontiguousarray(x[b])
        m["xo"] = np.ascontiguousarray(x[b][rows])
        m["po"] = np.ascontiguousarray(p[b][rows])
        m["masks"] = masks.reshape(128, 1024)
        m["sel"] = sel
        in_maps.append(m)
    nc = build_program()
    res = run_bass_kernel_spmd(nc, in_maps, core_ids=list(range(8)))
    if DBG:
        LAST['res'] = res.results
        LAST['own_rows'] = own_rows
    out = np.zeros((2, 8192, 1024), np.float32)
    for c in range(8):
        b, rows = own_rows[c]
        out[b, rows] = np.asarray(res.results[c]["out"], np.float32)
    return out
```

```python
import numpy as np
import concourse.bass as bass
import concourse.mybir as mybir
from concourse.bass_utils import run_bass_kernel_spmd
from contextlib import ExitStack
import os
STOP = int(os.environ.get('KSTOP', '99'))
DBG = os.environ.get('KDBG', '') != ''
LAST = {}

F32 = mybir.dt.float32
BF16 = mybir.dt.bfloat16
AF = mybir.ActivationFunctionType
ALU = mybir.AluOpType
AX = mybir.AxisListType

ENG = ['pe', 'act', 'dve', 'pool', 'sp']
EOBJ = {'pe': 'tensor', 'act': 'scalar', 'dve': 'vector', 'pool': 'gpsimd', 'sp': 'sync'}
NDS = 8
EPS = 1e-6


class Prog:
    def __init__(self, nc, es):
        self.nc = nc
        self.ops = {e: [] for e in ENG}
        self.cnt = {e: 0 for e in ENG}
        self.sem = {e: es.enter_context(nc.semaphore("s_" + e)) for e in ENG}
        self.seen = {e: {} for e in ENG}
        self.dq = ('sp', 'pool', 'act')
        self.dsem = {q: [es.enter_context(nc.semaphore("d_%s%d" % (q, i))) for i in range(NDS)] for q in self.dq}
        self.dcnt = {q: 0 for q in self.dq}
        self.last_w = {}
        self.readers = {}

    def need(self, eng, tok):
        kind, src, n = tok
        if kind == 'e':
            if src == eng and eng == 'pe':
                return
            key = ('e', src)
            val = n
            sem = self.sem[src]
        else:
            slot = n % NDS
            key = ('d', src, slot)
            val = 16 * (n // NDS + 1)
            sem = self.dsem[src][slot]
        if self.seen[eng].get(key, 0) >= val:
            return
        self.seen[eng][key] = val
        self.ops[eng].append(('wait', sem, val))

    def _deps(self, eng, r, w):
        toks = []
        for k in r:
            if k in self.last_w:
                toks.append(self.last_w[k])
        for k in w:
            if k in self.last_w:
                toks.append(self.last_w[k])
            toks.extend(self.readers.get(k, {}).values())
        for t in toks:
            self.need(eng, t)

    def _mark(self, tok, r, w):
        for k in r:
            d = self.readers.setdefault(k, {})
            if tok[0] == 'e':
                d[('e', tok[1])] = tok
            else:
                d[tok] = tok
        for k in w:
            self.last_w[k] = tok
            self.readers[k] = {}

    def op(self, eng, fn, r=(), w=()):
        self._deps(eng, r, w)
        n = self.cnt[eng] + 1
        self.cnt[eng] = n
        self.ops[eng].append(('op', fn))
        self._mark(('e', eng, n), r, w)

    def dma(self, q, out_ap, in_ap, r=(), w=()):
        self._deps(q, r, w)
        i = self.dcnt[q]
        self.dcnt[q] = i + 1
        if i >= NDS:
            self.need(q, ('d', q, i - NDS))
        self.ops[q].append(('dma', out_ap, in_ap, i % NDS))
        self._mark(('d', q, i), r, w)

    def barrier(self):
        for e in ENG:
            for q in self.dq:
                for i in range(max(0, self.dcnt[q] - NDS), self.dcnt[q]):
                    self.need(e, ('d', q, i))
            for f in ENG:
                if f != e and self.cnt[f] > 0:
                    self.need(e, ('e', f, self.cnt[f]))
        self.last_w = {}
        self.readers = {}

    def emit(self):
        nc = self.nc
        with nc.Block() as block:
            for e in ENG:
                items = self.ops[e]

                def body(eng, e=e, items=items):
                    for item in items:
                        if item[0] == 'wait':
                            eng.wait_ge(item[1], item[2])
                        elif item[0] == 'op':
                            item[1](eng).then_inc(self.sem[e], 1)
                        else:
                            _, o, i_, slot = item
                            eng.dma_start(out=o, in_=i_).then_inc(self.dsem[e][slot], 16)
                getattr(block, EOBJ[e])(body)
        self.ops = {e: [] for e in ENG}


V_GMIX, V_GMOE, V_GPL, V_CW, V_CB, V_BA, V_BX, V_LAM, V_BM, V_BGU = 0, 8, 16, 24, 56, 64, 72, 80, 88, 104
NV = 104 + 512


def build_program():
    nc = bass.Bass("TRN2", target_bir_lowering=False)

    def din(name, shape, dt=F32):
        return nc.dram_tensor(name, shape, dt, kind="ExternalInput").ap()

    def dscr(name, shape, dt):
        return nc.dram_tensor(name, shape, dt, kind=("ExternalOutput" if DBG else "Internal")).ap()

    xb = din("xb", [8192, 1024]); xo = din("xo", [2048, 1024]); po = din("po", [2048, 256])
    masks_d = din("masks", [128, 1024]); sel_d = din("sel", [128, 8])
    ident_d = din("ident", [128, 128]); ntri_d = din("ntri", [128, 128]); vecs_d = din("vecs", [128, NV])
    w_in = din("w_in", [1024, 3584]); lru_wa = din("lru_wa", [8, 128, 128]); lru_wx = din("lru_wx", [8, 128, 128])
    w_att_out = din("w_att_out", [512, 1024]); w_lru_out = din("w_lru_out", [1024, 1024])
    w_merge = din("w_merge", [1024, 2048]); w_out = din("w_out", [1024, 1024])
    w_router = din("w_router", [1024, 32]); b_router = din("b_router", [1, 32])
    w_gu = din("w_gate_up", [32, 1024, 2048]); w_dn = din("w_down", [32, 1024, 1024]); b_dn = din("b_down", [32, 1024])
    w_plg = din("w_pl_gate", [1024, 1024]); b_plg = din("b_pl_gate", [1, 1024]); w_plp = din("w_pl_proj", [256, 1024])
    g_post = din("g_post", [1, 1024]); g_fin = din("g_fin", [1, 1024])
    out_d = nc.dram_tensor("out", [2048, 1024], F32, kind="ExternalOutput").ap()

    kT_d = dscr("kT_d", [4, 128, 8192], BF16); v_d = dscr("v_d", [64, 128, 512], BF16)
    hown_d = dscr("hown_d", [128, 8, 2048], BF16); qT_d = dscr("qT_d", [4, 128, 2048], BF16)
    hg_d = dscr("hg_d", [128, 8, 2048], BF16); gm_d = dscr("gm_d", [128, 16, 2048], BF16)
    attT_d = dscr("attT_d", [128, 4, 2048], BF16); h_d = dscr("h_d", [2048, 1024], F32)

    with ExitStack() as es:
        P = Prog(nc, es)
        _uid = [0]
        _orig_sbuf = nc.sbuf_tensor

        def _sbuf(name, shape, dt):
            _uid[0] += 1
            return _orig_sbuf("%s_%d" % (name, _uid[0]), shape, dt)
        PB = [es.enter_context(nc.psum_tensor("pb%d" % i, [128, 512], F32)) for i in range(6)]
        PTs = [es.enter_context(nc.psum_tensor("ptb%d" % i, [128, 1024], BF16)) for i in range(2)]
        PT = PTs[0]

        def sbp(name, shape, dt=F32):
            return es.enter_context(_sbuf(name, shape, dt))
        vec = sbp("vec", [128, NV]); identf = sbp("identf", [128, 128]); identb = sbp("identb", [128, 128], BF16)
        cL = sbp("cL", [128, 8]); tmp8 = sbp("tmp8", [128, 8])
        P.dma('sp', vec[:], vecs_d[:, :], w=['vec'])
        P.dma('sp', identf[:], ident_d[:, :], w=['identf'])
        P.op('dve', lambda e: e.tensor_copy(out=identb[:], in_=identf[:]), r=['identf'], w=['identb'])
        P.op('act', lambda e: e.activation(out=tmp8[:], in_=vec[:, V_LAM:V_LAM + 8], func=AF.Exp, scale=-1.0), r=['vec'], w=['tmp8'])
        P.op('act', lambda e: e.activation(out=tmp8[:], in_=tmp8[:], func=AF.Ln, bias=1.0), r=['tmp8'], w=['tmp8'])
        P.op('dve', lambda e: e.tensor_scalar(out=cL[:], in0=tmp8[:], scalar1=-8.0, scalar2=None, op0=ALU.mult), r=['tmp8'], w=['cL'])
        P.emit()

        def front_end(S, src_rows, gcol, nblk, tag, next_rows=None, first=True):
            xt, xs, xnT, junk, ss, ms, sq, rstd = S['xt'], S['xs'], S['xnT'], S['junk'], S['ss'], S['ms'], S['sq'], S['rstd']
            if first:
                P.dma('sp', xt[:, 0:nblk, :], src_rows.rearrange("(j p) d -> p j d", p=128), w=['xt'])
            for j in range(nblk):
                P.op('act', lambda e, j=j: e.activation(out=junk[:], in_=xt[:, j, :], func=AF.Square, accum_out=ss[:, j:j + 1]),
                     r=['xt'], w=['junk', ('ss', j)])
            P.op('dve', lambda e: e.tensor_scalar(out=ms[:, 0:nblk], in0=ss[:, 0:nblk], scalar1=1.0 / 1024, scalar2=EPS, op0=ALU.mult, op1=ALU.add),
                 r=[('ss', j) for j in range(nblk)], w=['ms'])
            P.op('act', lambda e: e.activation(out=sq[:, 0:nblk], in_=ms[:, 0:nblk], func=AF.Sqrt), r=['ms'], w=['sq'])
            P.op('dve', lambda e: e.reciprocal(out=rstd[:, 0:nblk], in_=sq[:, 0:nblk]), r=['sq'], w=['rstd'])
            for j in range(nblk):
                P.op('act', lambda e, j=j: e.activation(out=xs[:, j, :], in_=xt[:, j, :], func=AF.Copy, scale=rstd[:, j:j + 1]),
                     r=['xt', 'rstd'], w=[('xs', j)])
            if next_rows is not None:
                P.dma('act', xt[:, 0:nblk, :], next_rows.rearrange("(j p) d -> p j d", p=128), w=['xt'])
            for dc in range(8):
                half = dc % 2
                for j in range(nblk):
                    P.op('pe', lambda e, j=j, dc=dc, half=half: e.transpose(PTs[half][:, j * 128:(j + 1) * 128],
                                                                            xs[:, j, dc * 128:(dc + 1) * 128], identb[:]),
                         r=[('xs', j), 'identb'], w=[('pt', half)])
                P.op('dve', lambda e, dc=dc, half=half: e.tensor_scalar(out=xnT[:, dc, 0:nblk * 128], in0=PTs[half][:, 0:nblk * 128],
                                                                         scalar1=vec[:, gcol + dc:gcol + dc + 1], scalar2=None, op0=ALU.mult),
                     r=[('pt', half)], w=[('xnT', dc)])

        def alloc_front(ph, nblk):
            S = {}
            S['xt'] = ph.enter_context(_sbuf("xt", [128, nblk, 1024], F32))
            S['xs'] = ph.enter_context(_sbuf("xs", [128, nblk, 1024], BF16))
            S['xnT'] = ph.enter_context(_sbuf("xnT", [128, 8, nblk * 128], BF16))
            S['junk'] = ph.enter_context(_sbuf("junk", [128, 1024], BF16))
            for nm in ('ss', 'ms', 'sq', 'rstd'):
                S[nm] = ph.enter_context(_sbuf(nm, [128, 4], F32))
            return S

        def load_w_bf16(dst, src, row0, nrows_chunks, col0, ncols, key):
            for c in range(nrows_chunks):
                for s0 in range(0, ncols, 1024):
                    n = min(1024, ncols - s0)
                    P.dma('pool', dst[:, c, s0:s0 + n], src[row0 + c * 128: row0 + (c + 1) * 128, col0 + s0: col0 + s0 + n], w=[(key, c)])

        def finish_early():
            with ExitStack() as phx:
                tt = phx.enter_context(_sbuf("early", [128, 1024], F32))
                P.dma('sp', tt[:], xo[0:128, :], w=['early'])
                P.dma('sp', out_d[0:128, :], tt[:], r=['early'], w=['early_o'])
                P.barrier()
                P.emit()
            return nc

        with ExitStack() as ph:
            sb = lambda name, shape, dt=F32: ph.enter_context(_sbuf(name, shape, dt))
            S = alloc_front(ph, 4)
            wkvu = sb("wkvu", [128, 8, 2048], BF16)
            wa = sb("wa", [128, 8, 128], BF16); wx = sb("wx", [128, 8, 128], BF16)
            selt = sb("selt", [128, 8])
            uext = sb("uext", [128, 8, 515]); hT = sb("hT", [128, 8, 512]); hlast = sb("hlast", [128, 8])
            uc = [sb("uc%d" % i, [128, 512]) for i in range(8)]
            ucb = [sb("ucb%d" % i, [128, 512], BF16) for i in range(2)]
            rr = [sb("rr%d" % i, [128, 512]) for i in range(8)]
            ii = [sb("ii%d" % i, [128, 512]) for i in range(8)]
            aa = [sb("aa%d" % i, [128, 512]) for i in range(8)]
            kst = [sb("kst%d" % i, [128, 512], BF16) for i in range(2)]
            vst = [sb("vst%d" % i, [128, 512], BF16) for i in range(2)]
            hst = sb("hst", [128, 8, 128], BF16)
            load_w_bf16(wkvu, w_in, 0, 8, 512, 2048, 'wkvu')
            P.dma('pool', wa[:], lru_wa.rearrange("n c d -> c n d"), w=['wa'])
            P.dma('pool', wx[:], lru_wx.rearrange("n c d -> c n d"), w=['wx'])
            P.dma('sp', selt[:], sel_d[:, :], w=['selt'])
            P.op('pool', lambda e: e.memset(uext[:, :, 0:3], 0.0), w=[('halo', c_) for c_ in range(8)])
            for tc in range(16):
                front_end(S, xb[tc * 512:(tc + 1) * 512, :], V_GMIX, 4, 'L', next_rows=(xb[(tc + 1) * 512:(tc + 2) * 512, :] if tc < 15 else None), first=(tc == 0))
                xnT = S['xnT']
                xk = [('xnT', dc) for dc in range(8)]
                def kgroup(pair):
                    pb = PB[2 + pair % 2]
                    for dc in range(8):
                        P.op('pe', lambda e, dc=dc: e.matmul(pb[:], lhsT=wkvu[:, dc, pair * 128:(pair + 1) * 128], rhs=xnT[:, dc, :],
                                                             start=(dc == 0), stop=(dc == 7)),
                             r=[('wkvu', dc), ('xnT', dc)], w=[('pb', 2 + pair % 2)])
                    P.op('act', lambda e: e.copy(out=kst[pair % 2][:], in_=pb[:]), r=[('pb', 2 + pair % 2)], w=[('kst', pair % 2)])
                    P.dma('sp', kT_d[pair, :, tc * 512:(tc + 1) * 512], kst[pair % 2][:], r=[('kst', pair % 2)], w=[('kT_d', pair, tc)])

                def vgroup(j):
                    pb = PB[4 + j % 2]
                    for dc in range(8):
                        P.op('pe', lambda e, dc=dc: e.matmul(pb[:], lhsT=xnT[:, dc, j * 128:(j + 1) * 128], rhs=wkvu[:, dc, 512:1024],
                                                             start=(dc == 0), stop=(dc == 7)),
                             r=[('wkvu', dc), ('xnT', dc)], w=[('pb', 4 + j % 2)])
                    P.op('dve', lambda e: e.tensor_copy(out=vst[j % 2][:], in_=pb[:]), r=[('pb', 4 + j % 2)], w=[('vst', j % 2)])
                    P.dma('sp', v_d[tc * 4 + j, :, :], vst[j % 2][:], r=[('vst', j % 2)], w=[('v_d', tc * 4 + j)])

                for cc in range(8):
                    pb = PB[cc % 2]
                    for dc in range(8):
                        P.op('pe', lambda e, pb=pb, dc=dc, cc=cc: e.matmul(pb[:], lhsT=wkvu[:, dc, 1024 + cc * 128:1024 + (cc + 1) * 128], rhs=xnT[:, dc, :],
                                                                           start=(dc == 0), stop=(dc == 7)),
                             r=[('wkvu', dc), ('xnT', dc)], w=[('pb', cc % 2)])
                    P.op('act', lambda e, pb=pb, cc=cc: e.copy(out=uext[:, cc, 3:515], in_=pb[:]), r=[('pb', cc % 2)], w=[('uext', cc)])

                kvq = [lambda p_=p_: kgroup(p_) for p_ in range(4)] + [lambda j_=j_: vgroup(j_) for j_ in range(4)]

                def s1(cc):
                    kU = [('uext', cc), ('halo', cc)]
                    P.op('dve', lambda e: e.tensor_scalar(out=uc[cc][:], in0=uext[:, cc, 3:515], scalar1=vec[:, V_CW + 24 + cc:V_CW + 25 + cc],
                                                          scalar2=vec[:, V_CB + cc:V_CB + cc + 1], op0=ALU.mult, op1=ALU.add),
                         r=kU, w=[('uc', cc)])
                    for jj in range(3):
                        P.op('dve', lambda e, jj=jj: e.scalar_tensor_tensor(out=uc[cc][:], in0=uext[:, cc, jj:jj + 512],
                                                                            scalar=vec[:, V_CW + jj * 8 + cc:V_CW + jj * 8 + cc + 1],
                                                                            in1=uc[cc][:], op0=ALU.mult, op1=ALU.add),
                             r=kU + [('uc', cc)], w=[('uc', cc)])
                    P.op('pool', lambda e: e.tensor_copy(out=uext[:, cc, 0:3], in_=uext[:, cc, 512:515]), r=kU, w=kU)
                    b = cc % 2
                    P.op('act', lambda e: e.copy(out=ucb[b][:], in_=uc[cc][:]), r=[('uc', cc)], w=[('ucb', b)])
                    P.op('pe', lambda e: e.matmul(PB[0][:], lhsT=wa[:, cc, :], rhs=ucb[b][:], start=True, stop=True), r=['wa', ('ucb', b)], w=[('pb', 0)])
                    P.op('act', lambda e: e.activation(out=rr[cc][:], in_=PB[0][:], func=AF.Sigmoid, bias=vec[:, V_BA + cc:V_BA + cc + 1]),
                         r=[('pb', 0)], w=[('rr', cc)])
                    P.op('pe', lambda e: e.matmul(PB[1][:], lhsT=wx[:, cc, :], rhs=ucb[b][:], start=True, stop=True), r=['wx', ('ucb', b)], w=[('pb', 1)])
                    P.op('act', lambda e: e.activation(out=ii[cc][:], in_=PB[1][:], func=AF.Sigmoid, bias=vec[:, V_BX + cc:V_BX + cc + 1]),
                         r=[('pb', 1)], w=[('ii', cc)])

                def s2(cc):
                    P.op('act', lambda e: e.activation(out=aa[cc][:], in_=rr[cc][:], func=AF.Exp, scale=cL[:, cc:cc + 1]), r=[('rr', cc), 'cL'], w=[('aa', cc)])
                    P.op('pool', lambda e: e.tensor_tensor(out=rr[cc][:], in0=aa[cc][:], in1=aa[cc][:], op=ALU.mult), r=[('aa', cc)], w=[('rr', cc)])
                    P.op('pool', lambda e: e.tensor_tensor(out=ii[cc][:], in0=ii[cc][:], in1=uc[cc][:], op=ALU.mult), r=[('ii', cc), ('uc', cc)], w=[('ii', cc)])

                def s3(cc):
                    P.op('act', lambda e: e.activation(out=rr[cc][:], in_=rr[cc][:], func=AF.Sqrt, scale=-1.0, bias=1.0), r=[('rr', cc)], w=[('rr', cc)])
                    P.op('pool', lambda e: e.tensor_tensor(out=ii[cc][:], in0=ii[cc][:], in1=rr[cc][:], op=ALU.mult), r=[('ii', cc), ('rr', cc)], w=[('ii', cc)])

                def s4(cc):
                    if tc == 0:
                        P.op('dve', lambda e: e.tensor_tensor_scan(out=hT[:, cc, :], data0=aa[cc][:], data1=ii[cc][:], initial=0.0, op0=ALU.mult, op1=ALU.add),
                             r=[('aa', cc), ('ii', cc)], w=[('hT', cc)])
                    else:
                        P.op('dve', lambda e: e.tensor_tensor_scan(out=hT[:, cc, :], data0=aa[cc][:], data1=ii[cc][:], initial=hlast[:, cc:cc + 1],
                                                                   op0=ALU.mult, op1=ALU.add),
                             r=[('aa', cc), ('ii', cc), ('hlast', cc)], w=[('hT', cc)])
                    P.op('dve', lambda e: e.tensor_copy(out=hlast[:, cc:cc + 1], in_=hT[:, cc, 511:512]), r=[('hT', cc)], w=[('hlast', cc)])
                    so = 4 * (tc % 2)
                    P.op('dve', lambda e: e.tensor_scalar(out=hst[:, cc, :], in0=hT[:, cc, 0:128], scalar1=selt[:, so:so + 1], scalar2=None, op0=ALU.mult),
                         r=[('hT', cc), 'selt'], w=[('hst', cc)])
                    for pp in range(1, 4):
                        P.op('dve', lambda e, pp=pp: e.scalar_tensor_tensor(out=hst[:, cc, :], in0=hT[:, cc, pp * 128:(pp + 1) * 128],
                                                                            scalar=selt[:, so + pp:so + pp + 1], in1=hst[:, cc, :],
                                                                            op0=ALU.mult, op1=ALU.add),
                             r=[('hT', cc), ('hst', cc)], w=[('hst', cc)])

                h0 = range(0, 4); h1 = range(4, 8)
                for cc in h0:
                    s1(cc); kvq[cc]()
                for cc in h0:
                    s2(cc)
                for cc in h1:
                    s1(cc)
                for cc in h1:
                    kvq[cc]()
                for cc in h0:
                    s3(cc)
                for cc in h0:
                    s4(cc)
                for cc in h1:
                    s2(cc)
                for cc in h1:
                    s3(cc)
                for cc in h1:
                    s4(cc)
                P.dma('sp', hown_d[:, :, tc * 128:(tc + 1) * 128], hst[:], r=[('hst', cc) for cc in range(8)], w=[('hown_d', tc)])
            P.barrier()
            P.emit()

        if STOP == 1:
            return finish_early()
        with ExitStack() as ph:
            sb = lambda name, shape, dt=F32: ph.enter_context(_sbuf(name, shape, dt))
            S = alloc_front(ph, 4)
            wqg = sb("wqg", [128, 8, 1536], BF16); wm = sb("wm", [128, 8, 2048], BF16)
            hoc = sb("hoc", [128, 8, 512], BF16); hg = sb("hg", [128, 8, 512], BF16); gm = sb("gm", [128, 16, 512], BF16)
            qst = [sb("qst%d" % i, [128, 512], BF16) for i in range(2)]
            gl = [sb("gl%d" % i, [128, 512]) for i in range(2)]
            load_w_bf16(wqg[:, :, 0:512], w_in, 0, 8, 0, 512, 'wq')
            load_w_bf16(wqg[:, :, 512:1536], w_in, 0, 8, 2560, 1024, 'wg')
            load_w_bf16(wm, w_merge, 0, 8, 0, 2048, 'wm')
            for oc in range(4):
                front_end(S, xo[oc * 512:(oc + 1) * 512, :], V_GMIX, 4, 'O', next_rows=(xo[(oc + 1) * 512:(oc + 2) * 512, :] if oc < 3 else None), first=(oc == 0))
                xnT = S['xnT']
                P.dma('sp', hoc[:], hown_d[:, :, oc * 512:(oc + 1) * 512], w=['hoc'])
                for pair in range(4):
                    pb = PB[2 + pair % 2]
                    for dc in range(8):
                        P.op('pe', lambda e, pb=pb, dc=dc, pair=pair: e.matmul(pb[:], lhsT=wqg[:, dc, pair * 128:(pair + 1) * 128], rhs=xnT[:, dc, :],
                                                                               start=(dc == 0), stop=(dc == 7)),
                             r=[('wq', dc), ('xnT', dc)], w=[('pb', 2 + pair % 2)])
                    P.op('act', lambda e, pb=pb, pair=pair: e.activation(out=qst[pair % 2][:], in_=pb[:], func=AF.Copy, scale=0.125),
                         r=[('pb', 2 + pair % 2)], w=[('qst', pair % 2)])
                    P.dma('sp', qT_d[pair, :, oc * 512:(oc + 1) * 512], qst[pair % 2][:], r=[('qst', pair % 2)], w=[('qT_d', pair, oc)])
                for cc in range(8):
                    pb = PB[cc % 2]
                    for dc in range(8):
                        P.op('pe', lambda e, pb=pb, dc=dc, cc=cc: e.matmul(pb[:], lhsT=wqg[:, dc, 512 + cc * 128:512 + (cc + 1) * 128], rhs=xnT[:, dc, :],
                                                                           start=(dc == 0), stop=(dc == 7)),
                             r=[('wg', dc), ('xnT', dc)], w=[('pb', cc % 2)])
                    P.op('act', lambda e, pb=pb, cc=cc: e.activation(out=gl[cc % 2][:], in_=pb[:], func=AF.Gelu), r=[('pb', cc % 2)], w=[('gl', cc % 2)])
                    P.op('dve', lambda e, cc=cc: e.tensor_tensor(out=hg[:, cc, :], in0=gl[cc % 2][:], in1=hoc[:, cc, :], op=ALU.mult),
                         r=[('gl', cc % 2), 'hoc'], w=['hg'])
                P.dma('sp', hg_d[:, :, oc * 512:(oc + 1) * 512], hg[:], r=['hg'], w=[('hg_d', oc)])
                for c in range(16):
                    pb = PB[4 + c % 2]
                    for dc in range(8):
                        P.op('pe', lambda e, pb=pb, dc=dc, c=c: e.matmul(pb[:], lhsT=wm[:, dc, c * 128:(c + 1) * 128], rhs=xnT[:, dc, :],
                                                                         start=(dc == 0), stop=(dc == 7)),
                             r=[('wm', dc), ('xnT', dc)], w=[('pb', 4 + c % 2)])
                    P.op('act', lambda e, pb=pb, c=c: e.activation(out=gm[:, c, :], in_=pb[:], func=AF.Sigmoid, bias=vec[:, V_BM + c:V_BM + c + 1]),
                         r=[('pb', 4 + c % 2)], w=['gm'])
                P.dma('sp', gm_d[:, :, oc * 512:(oc + 1) * 512], gm[:], r=['gm'], w=[('gm_d', oc)])
            P.barrier()
            P.emit()

        if STOP == 2:
            return finish_early()
        with ExitStack() as ph:
            sb = lambda name, shape, dt=F32: ph.enter_context(_sbuf(name, shape, dt))
            kT = sb("kT", [128, 4, 8192], BF16); vv = sb("vv", [128, 64, 512], BF16); qT = sb("qTp", [128, 8, 2048], BF16)
            maskt = sb("maskt", [128, 1024]); ntri = sb("ntri", [128, 128]); ntrib = sb("ntrib", [128, 128], BF16)
            ones2 = sb("ones2", [128, 2]); ones2b = sb("ones2b", [128, 2], BF16)
            e_t = [sb("e_t%d" % i, [128, 512]) for i in range(2)]
            sp_t = [sb("sp_t%d" % i, [128, 512]) for i in range(2)]
            sphi = [sb("sphi%d" % i, [128, 512], BF16) for i in range(4)]
            splo = [sb("splo%d" % i, [128, 512], BF16) for i in range(4)]
            w_t = [sb("w_t%d" % i, [128, 512], BF16) for i in range(2)]
            Xc = [sb("Xc%d" % i, [128, 4]) for i in range(3)]
            Rb = [sb("Rb%d" % i, [128, 1]) for i in range(4)]
            Eself = sb("Eself", [128, 4, 6]); Esel = sb("Esel", [128, 4, 6], BF16)
            f_t = [sb("f_t%d" % i, [128, 4]) for i in range(3)]
            oacc = [sb("oacc%d" % i, [128, 2, 64]) for i in range(2)]
            att = [sb("att%d" % i, [128, 512], BF16) for i in range(2)]
            attT = [sb("attT%d" % i, [128, 4, 128], BF16) for i in range(2)]
            P.op('pool', lambda e: e.memset(qT[:], 0.0), w=[('qT', h_) for h_ in range(8)])
            for pair in range(4):
                for hh in range(2):
                    P.dma('sp', kT[:, pair, hh * 4096:(hh + 1) * 4096], kT_d[pair, :, hh * 4096:(hh + 1) * 4096], w=[('kT', pair, hh)])
                for half in range(2):
                    P.dma('sp', qT[half * 64:(half + 1) * 64, 2 * pair + half, :], qT_d[pair, half * 64:(half + 1) * 64, :], w=[('qT', 2 * pair + half)])
            for b8 in range(8):
                P.dma('sp', vv[:, b8 * 8:(b8 + 1) * 8, :], v_d[b8 * 8:(b8 + 1) * 8, :, :].rearrange("b p c -> p b c"), w=[('vv', b8)])
            P.dma('sp', maskt[:], masks_d[:, :], w=['maskt'])
            P.dma('sp', ntri[:], ntri_d[:, :], w=['ntri'])
            P.op('dve', lambda e: e.tensor_copy(out=ntrib[:], in_=ntri[:]), r=['ntri'], w=['ntrib'])
            P.op('pool', lambda e: e.memset(ones2[:], 1.0), w=['ones2'])
            P.op('dve', lambda e: e.tensor_copy(out=ones2b[:], in_=ones2[:]), r=['ones2'], w=['ones2b'])
            P.op('pool', lambda e: e.memset(Eself[:], 0.0), w=['Eself'])
            for s_ in range(4):
                if s_ >= 1:
                    P.op('pool', lambda e, s_=s_: e.memset(Eself[:, s_, 0:s_], 1.0), r=['Eself'], w=['Eself'])
                P.op('pool', lambda e, s_=s_: e.memset(Eself[:, s_, 4:5], 1.0), r=['Eself'], w=['Eself'])
            P.op('dve', lambda e: e.tensor_copy(out=Esel[:], in_=Eself[:]), r=['Eself'], w=['Esel'])

            groups = []
            for slot in range(16):
                ng = (slot // 2) * 2 + 1 + (slot % 2)
                for h in range(8):
                    for g in range(ng):
                        groups.append((slot, h, g, ng))
            NG = len(groups)

            def zmm(gi, bank, closed):
                slot, h, g, ng = groups[gi]
                pair = h // 2
                base = 4 * ng - 4 * (g + 1)
                for s in range(4):
                    kb = base + s
                    st = True if closed else (s == 0)
                    sp_ = True if closed else False
                    P.op('pe', lambda e, s=s, kb=kb, pair=pair, h=h, slot=slot, st=st, sp_=sp_: e.matmul(
                        PB[bank][:, s * 128:(s + 1) * 128], lhsT=kT[:, pair, kb * 128:(kb + 1) * 128],
                        rhs=qT[:, h, slot * 128:(slot + 1) * 128], start=st, stop=sp_),
                        r=[('kT', pair, kb // 32), ('qT', h)], w=[('pb', bank)])

            def stageA1(gi):
                slot, h, g, ng = groups[gi]
                za = gi % 2
                zmm(gi, za, True)
                P.op('act', lambda e: e.activation(out=e_t[gi % 2][:], in_=PB[za][:], func=AF.Exp), r=[('pb', za)], w=[('e_t', gi % 2)])

            def stageA2(gi):
                slot, h, g, ng = groups[gi]
                P.op('act', lambda e: e.activation(out=sp_t[gi % 2][:], in_=e_t[gi % 2][:], func=AF.Ln, bias=1.0), r=[('e_t', gi % 2)], w=[('sp_t', gi % 2)])
                if g == 0:
                    mo = 512 * (slot % 2)
                    P.op('dve', lambda e, mo=mo: e.tensor_tensor(out=sp_t[gi % 2][:], in0=sp_t[gi % 2][:], in1=maskt[:, mo:mo + 512], op=ALU.mult),
                         r=[('sp_t', gi % 2), 'maskt'], w=[('sp_t', gi % 2)])
                P.op('dve', lambda e: e.tensor_copy(out=sphi[gi % 4][:], in_=sp_t[gi % 2][:]), r=[('sp_t', gi % 2)], w=[('sphi', gi % 4)])
                P.op('pool', lambda e: e.tensor_tensor(out=splo[gi % 4][:], in0=sp_t[gi % 2][:], in1=sphi[gi % 4][:], op=ALU.subtract),
                     r=[('sp_t', gi % 2), ('sphi', gi % 4)], w=[('splo', gi % 4)])

            def stageB1(gi):
                slot, h, g, ng = groups[gi]
                ab = 2 + gi % 2
                zmm(gi, ab, False)
                P.op('pe', lambda e: e.matmul(PB[ab][:], lhsT=ntrib[:], rhs=sphi[gi % 4][:], start=False, stop=False),
                     r=[('sphi', gi % 4), 'ntrib'], w=[('pb', ab)])
                P.op('pe', lambda e: e.matmul(PB[ab][:], lhsT=ntrib[:], rhs=splo[gi % 4][:], start=False, stop=True),
                     r=[('splo', gi % 4), 'ntrib'], w=[('pb', ab)])
                P.op('act', lambda e: e.activation(out=w_t[gi % 2][:], in_=PB[ab][:], func=AF.Exp), r=[('pb', ab)], w=[('w_t', gi % 2)])

            def stageB2(gi):
                slot, h, g, ng = groups[gi]
                for s in range(4):
                    P.op('pe', lambda e, s=s: e.matmul(PB[4][:, 0:6], lhsT=sphi[gi % 4][:, s * 128:(s + 1) * 128], rhs=Esel[:, s, :], start=(s == 0), stop=False),
                         r=[('sphi', gi % 4), 'Esel'], w=[('pb', 4)])
                    P.op('pe', lambda e, s=s: e.matmul(PB[4][:, 0:6], lhsT=splo[gi % 4][:, s * 128:(s + 1) * 128], rhs=Esel[:, s, :], start=False, stop=(s == 3)),
                         r=[('splo', gi % 4), 'Esel'], w=[('pb', 4)])
                if g == 0:
                    mo = 512 * (slot % 2)
                    P.op('dve', lambda e, mo=mo: e.tensor_tensor(out=w_t[gi % 2][:], in0=w_t[gi % 2][:], in1=maskt[:, mo:mo + 512], op=ALU.mult),
                         r=[('w_t', gi % 2), 'maskt'], w=[('w_t', gi % 2)])
                R = Rb[gi % 4]; Rn = Rb[(gi + 1) % 4]
                X = Xc[gi % 3]
                if g == 0:
                    P.op('pool', lambda e, R=R: e.memset(R[:], 0.0), w=[('R', gi % 4)])
                P.op('dve', lambda e, R=R, X=X: e.tensor_scalar(out=X[:], in0=PB[4][:, 0:4], scalar1=R[:, 0:1], scalar2=None, op0=ALU.add),
                     r=[('pb', 4), ('R', gi % 4)], w=[('X', gi % 3)])
                if g != ng - 1:
                    P.op('dve', lambda e, R=R, Rn=Rn: e.tensor_scalar(out=Rn[:], in0=PB[4][:, 4:5], scalar1=R[:, 0:1], scalar2=None, op0=ALU.add),
                         r=[('pb', 4), ('R', gi % 4)], w=[('R', (gi + 1) % 4)])
                P.op('act', lambda e, X=X: e.activation(out=f_t[gi % 3][:], in_=X[:], func=AF.Exp, scale=-1.0),
                     r=[('X', gi % 3)], w=[('f_t', gi % 3)])

            def stageC(gi):
                slot, h, g, ng = groups[gi]
                base = 4 * ng - 4 * (g + 1)
                hidx = slot * 8 + h
                oa = oacc[hidx % 2]
                ok = ('oacc', hidx % 2)
                for s in range(4):
                    kb = base + s
                    P.op('pe', lambda e, s=s, kb=kb, h=h: e.matmul(PB[5][:, s * 64:(s + 1) * 64], lhsT=w_t[gi % 2][:, s * 128:(s + 1) * 128],
                                                                   rhs=vv[:, kb, h * 64:(h + 1) * 64], start=True, stop=True),
                         r=[('w_t', gi % 2), ('vv', kb // 8)], w=[('pb', 5)])
                for s in range(4):
                    okk = ('oacc', hidx % 2, s % 2)
                    if g == 0 and s < 2:
                        P.op('dve', lambda e, s=s, oa=oa: e.tensor_scalar(out=oa[:, s % 2, :], in0=PB[5][:, s * 64:(s + 1) * 64], scalar1=f_t[gi % 3][:, s:s + 1], scalar2=None, op0=ALU.mult),
                             r=[('pb', 5), ('f_t', gi % 3)], w=[okk])
                    else:
                        P.op('dve', lambda e, s=s, oa=oa: e.scalar_tensor_tensor(out=oa[:, s % 2, :], in0=PB[5][:, s * 64:(s + 1) * 64], scalar=f_t[gi % 3][:, s:s + 1],
                                                                                 in1=oa[:, s % 2, :], op0=ALU.mult, op1=ALU.add),
                             r=[('pb', 5), ('f_t', gi % 3), okk], w=[okk])
                if g == ng - 1:
                    P.op('pool', lambda e, oa=oa, h=h, slot=slot: e.tensor_tensor(out=att[slot % 2][:, h * 64:(h + 1) * 64], in0=oa[:, 0, :], in1=oa[:, 1, :], op=ALU.add),
                         r=[('oacc', hidx % 2, 0), ('oacc', hidx % 2, 1)], w=[('att', slot % 2, h)])
                    if h == 7:
                        for c4 in range(4):
                            P.op('pe', lambda e, c4=c4, slot=slot: e.transpose(PT[:, c4 * 128:(c4 + 1) * 128], att[slot % 2][:, c4 * 128:(c4 + 1) * 128], identb[:]),
                                 r=[('att', slot % 2, 2 * c4), ('att', slot % 2, 2 * c4 + 1), 'identb'], w=['ptA'])
                        P.op('dve', lambda e, slot=slot: e.tensor_copy(out=attT[slot % 2][:], in_=PT[:, 0:512].rearrange("p (c q) -> p c q", c=4)),
                             r=['ptA'], w=[('attT', slot % 2)])
                        P.dma('sp', attT_d[:, :, slot * 128:(slot + 1) * 128], attT[slot % 2][:], r=[('attT', slot % 2)], w=[('attT_d', slot)])

            for it in range(NG + 3):
                if it < NG:
                    stageA1(it)
                if 0 <= it - 2 < NG:
                    stageB1(it - 2)
                if it < NG:
                    stageA2(it)
                if 0 <= it - 2 < NG:
                    stageB2(it - 2)
                if 0 <= it - 3 < NG:
                    stageC(it - 3)
            P.barrier()
            P.emit()

        if STOP == 3:
            return finish_early()
        with ExitStack() as ph:
            sb = lambda name, shape, dt=F32: ph.enter_context(_sbuf(name, shape, dt))
            wao = sb("wao", [128, 4, 1024], BF16); wlo = sb("wlo", [128, 8, 1024], BF16); wo = sb("wo", [128, 8, 1024], BF16)
            attc = sb("attc", [128, 4, 512], BF16); hgc = sb("hgc", [128, 8, 512], BF16); gmc = sb("gmc", [128, 16, 512], BF16)
            t1 = [sb("t1%d" % i, [128, 512]) for i in range(2)]
            t2 = [sb("t2%d" % i, [128, 512]) for i in range(2)]
            mT = sb("mT", [128, 8, 512], BF16)
            xres = sb("xres", [128, 4, 1024]); hres = sb("hres", [128, 4, 1024])
            load_w_bf16(wao, w_att_out, 0, 4, 0, 1024, 'wao')
            load_w_bf16(wlo, w_lru_out, 0, 8, 0, 1024, 'wlo')
            load_w_bf16(wo, w_out, 0, 8, 0, 1024, 'wo')
            for oc in range(4):
                sl = slice(oc * 512, (oc + 1) * 512)
                P.dma('sp', attc[:], attT_d[:, :, sl], w=['attc'])
                P.dma('sp', hgc[:], hg_d[:, :, sl], w=['hgc'])
                P.dma('sp', gmc[:], gm_d[:, :, sl], w=['gmc'])
                P.dma('sp', xres[:], xo[sl, :].rearrange("(j p) d -> p j d", p=128), w=['xres'])
                for dmc in range(8):
                    pa = PB[(2 * dmc) % 4]; pl = PB[(2 * dmc + 1) % 4]
                    ka = ('pb', (2 * dmc) % 4); kl = ('pb', (2 * dmc + 1) % 4)
                    for cc in range(4):
                        P.op('pe', lambda e, pa=pa, cc=cc, dmc=dmc: e.matmul(pa[:], lhsT=wao[:, cc, dmc * 128:(dmc + 1) * 128], rhs=attc[:, cc, :],
                                                                             start=(cc == 0), stop=(cc == 3)), r=[('wao', cc), 'attc'], w=[ka])
                    for cc in range(8):
                        P.op('pe', lambda e, pl=pl, cc=cc, dmc=dmc: e.matmul(pl[:], lhsT=wlo[:, cc, dmc * 128:(dmc + 1) * 128], rhs=hgc[:, cc, :],
                                                                             start=(cc == 0), stop=(cc == 7)), r=[('wlo', cc), 'hgc'], w=[kl])
                    b = dmc % 2
                    P.op('dve', lambda e, pa=pa, dmc=dmc, b=b: e.tensor_tensor(out=t1[b][:], in0=pa[:], in1=gmc[:, dmc, :], op=ALU.mult), r=[ka, 'gmc'], w=[('t1', b)])
                    P.op('dve', lambda e, pl=pl, dmc=dmc, b=b: e.tensor_tensor(out=t2[b][:], in0=pl[:], in1=gmc[:, 8 + dmc, :], op=ALU.mult), r=[kl, 'gmc'], w=[('t2', b)])
                    P.op('pool', lambda e, dmc=dmc, b=b: e.tensor_tensor(out=mT[:, dmc, :], in0=t1[b][:], in1=t2[b][:], op=ALU.add),
                         r=[('t1', b), ('t2', b)], w=[('mT', dmc)])
                for j in range(4):
                    for n in range(2):
                        pi = 4 + (j * 2 + n) % 2
                        pb = PB[pi]
                        for dmc in range(8):
                            P.op('pe', lambda e, pb=pb, dmc=dmc, j=j, n=n: e.matmul(pb[:], lhsT=mT[:, dmc, j * 128:(j + 1) * 128], rhs=wo[:, dmc, n * 512:(n + 1) * 512],
                                                                                    start=(dmc == 0), stop=(dmc == 7)), r=[('mT', dmc), ('wo', dmc)], w=[('pb', pi)])
                        P.op('dve', lambda e, pb=pb, j=j, n=n: e.tensor_tensor(out=hres[:, j, n * 512:(n + 1) * 512], in0=pb[:], in1=xres[:, j, n * 512:(n + 1) * 512], op=ALU.add),
                             r=[('pb', pi), 'xres'], w=['hres'])
                P.dma('sp', h_d[sl, :].rearrange("(j p) d -> p j d", p=128), hres[:], r=['hres'], w=[('h_d', oc)])
            P.barrier()
            P.emit()

        if STOP == 4:
            return finish_early()
        with ExitStack() as ph:
            sb = lambda name, shape, dt=F32: ph.enter_context(_sbuf(name, shape, dt))
            phH = ExitStack()
            sbH = lambda name, shape, dt=F32: phH.enter_context(_sbuf(name, shape, dt))
            H = sbH("H", [128, 16, 1024])
            junkE = sbH("junkE", [128, 1024], BF16); ones1 = sbH("ones1", [1, 128])
            sm = {nm: sbH("e_" + nm, [128, 8]) for nm in ('ss', 'ms', 'sq', 'rstd', 'mx', 'negm', 'sum', 'rs')}
            xn2T = sb("xn2T", [128, 8, 2048], BF16)
            G = sb("G", [128, 16, 32])
            xsf = sb("xsf", [128, 1024]); xTf = sb("xTf", [128, 8, 128])
            wr = sb("wr", [128, 8, 32]); brt = sb("brt", [1, 32])
            lg = sb("lg", [128, 32]); mk = sb("mk", [128, 32]); ex = sb("ex", [128, 32])
            wgt = [sb("wgt%d" % i, [128, 8, 1024], BF16) for i in range(2)]
            wdt = [sb("wdt%d" % i, [128, 4, 1024], BF16) for i in range(2)]
            bdall = sb("bdall", [32, 1024]); GT = sb("GT", [32, 128])
            gc = [sb("gc%d" % i, [128, 512]) for i in range(2)]
            sg = [sb("sg%d" % i, [128, 512]) for i in range(2)]
            u1 = [sb("u1%d" % i, [128, 512]) for i in range(2)]
            actT = [sb("actT%d" % i, [128, 4, 512], BF16) for i in range(2)]
            for oc in range(4):
                P.dma('sp', H[:, oc * 4:(oc + 1) * 4, :], h_d[oc * 512:(oc + 1) * 512, :].rearrange("(j p) d -> p j d", p=128), w=[('H', oc * 4 + j) for j in range(4)])
            P.dma('sp', wr[:], w_router.rearrange("(c p) e -> p c e", p=128), w=['wr'])
            P.dma('sp', brt[:], b_router[:, :], w=['brt'])
            P.dma('sp', bdall[:], b_dn[:, :], w=['bdall'])
            P.op('pool', lambda e: e.memset(ones1[:], 1.0), w=['ones1'])
            for blk in range(16):
                P.op('act', lambda e, blk=blk: e.activation(out=junkE[:], in_=H[:, blk, :], func=AF.Square, accum_out=sm['ss'][:, 0:1]),
                     r=[('H', blk)], w=['junkE', 'ss'])
                P.op('dve', lambda e: e.tensor_scalar(out=sm['ms'][:, 0:1], in0=sm['ss'][:, 0:1], scalar1=1.0 / 1024, scalar2=EPS, op0=ALU.mult, op1=ALU.add), r=['ss'], w=['ms'])
                P.op('act', lambda e: e.activation(out=sm['sq'][:, 0:1], in_=sm['ms'][:, 0:1], func=AF.Sqrt), r=['ms'], w=['sq'])
                P.op('dve', lambda e: e.reciprocal(out=sm['rstd'][:, 0:1], in_=sm['sq'][:, 0:1]), r=['sq'], w=['rstd'])
                P.op('act', lambda e, blk=blk: e.activation(out=xsf[:], in_=H[:, blk, :], func=AF.Copy, scale=sm['rstd'][:, 0:1]), r=[('H', blk), 'rstd'], w=['xsf'])
                for dc in range(8):
                    pi = dc % 2
                    P.op('pe', lambda e, dc=dc, pi=pi: e.transpose(PB[pi][:, 0:128], xsf[:, dc * 128:(dc + 1) * 128], identf[:]), r=['xsf', 'identf'], w=[('pb', pi)])
                    P.op('dve', lambda e, dc=dc, pi=pi: e.tensor_scalar(out=xTf[:, dc, :], in0=PB[pi][:, 0:128], scalar1=vec[:, V_GMOE + dc:V_GMOE + dc + 1], scalar2=None, op0=ALU.mult),
                         r=[('pb', pi)], w=[('xTf', dc)])
                    P.op('pool', lambda e, dc=dc, blk=blk: e.tensor_copy(out=xn2T[:, dc, blk * 128:(blk + 1) * 128], in_=xTf[:, dc, :]), r=[('xTf', dc)], w=[('xn2T', blk)])
                for dc in range(8):
                    P.op('pe', lambda e, dc=dc: e.matmul(PB[2][:, 0:32], lhsT=xTf[:, dc, :], rhs=wr[:, dc, :], start=(dc == 0), stop=False),
                         r=[('xTf', dc), 'wr'], w=[('pb', 2)])
                P.op('pe', lambda e: e.matmul(PB[2][:, 0:32], lhsT=ones1[:], rhs=brt[:], start=False, stop=True), r=['ones1', 'brt'], w=[('pb', 2)])
                P.op('dve', lambda e: e.tensor_copy(out=lg[:], in_=PB[2][:, 0:32]), r=[('pb', 2)], w=['lg'])
                P.op('dve', lambda e: e.max(out=sm['mx'][:], in_=lg[:]), r=['lg'], w=['mx'])
                P.op('dve', lambda e: e.tensor_scalar(out=mk[:], in0=lg[:], scalar1=sm['mx'][:, 3:4], scalar2=None, op0=ALU.is_ge), r=['lg', 'mx'], w=['mk'])
                P.op('dve', lambda e: e.tensor_scalar(out=sm['negm'][:, 0:1], in0=sm['mx'][:, 0:1], scalar1=-1.0, scalar2=None, op0=ALU.mult), r=['mx'], w=['negm'])
                P.op('act', lambda e: e.activation(out=ex[:], in_=lg[:], func=AF.Exp, bias=sm['negm'][:, 0:1]), r=['lg', 'negm'], w=['ex'])
                P.op('dve', lambda e: e.tensor_tensor(out=ex[:], in0=ex[:], in1=mk[:], op=ALU.mult), r=['ex', 'mk'], w=['ex'])
                P.op('dve', lambda e: e.tensor_reduce(out=sm['sum'][:, 0:1], in_=ex[:], axis=AX.X, op=ALU.add), r=['ex'], w=['sum'])
                P.op('dve', lambda e: e.reciprocal(out=sm['rs'][:, 0:1], in_=sm['sum'][:, 0:1]), r=['sum'], w=['rs'])
                P.op('dve', lambda e, blk=blk: e.tensor_scalar(out=G[:, blk, :], in0=ex[:], scalar1=sm['rs'][:, 0:1], scalar2=None, op0=ALU.mult), r=['ex', 'rs'], w=[('G', blk)])
                P.op('pe', lambda e, blk=blk: e.transpose(PB[3][0:32, 0:128], G[:, blk, :], identf[:]), r=[('G', blk), 'identf'], w=[('pb', 3)])
                P.op('dve', lambda e: e.tensor_copy(out=GT[:], in_=PB[3][0:32, 0:128]), r=[('pb', 3)], w=['GT'])
                for n in range(2):
                    P.op('pe', lambda e, n=n: e.matmul(PB[4 + n][:], lhsT=GT[:], rhs=bdall[:, n * 512:(n + 1) * 512], start=True, stop=True), r=['GT', 'bdall'], w=[('pb', 4 + n)])
                    P.op('dve', lambda e, n=n, blk=blk: e.tensor_tensor(out=H[:, blk, n * 512:(n + 1) * 512], in0=PB[4 + n][:], in1=H[:, blk, n * 512:(n + 1) * 512], op=ALU.add),
                         r=[('pb', 4 + n), ('H', blk)], w=[('H', blk)])
            def load_expert(ei, hf):
                wb = (ei * 2 + hf) % 2
                for dc in range(8):
                    P.dma('pool', wgt[wb][:, dc, :].rearrange("p (a f) -> p a f", a=2),
                          w_gu[ei, dc * 128:(dc + 1) * 128, :].rearrange("p (a f) -> p a f", a=2)[:, :, hf * 512:(hf + 1) * 512], w=[('wgt', wb, dc)])
                for fc in range(4):
                    P.dma('pool', wdt[wb][:, fc, :], w_dn[ei, hf * 512 + fc * 128: hf * 512 + (fc + 1) * 128, :], w=[('wdt', wb, fc)])

            seq = [(ei, hf) for ei in range(32) for hf in range(2)]

            def GU(si, tg, ab):
                ei, hf = seq[si]
                wb = si % 2
                for fc in range(4):
                    pg = PB[(2 * fc) % 4]; pu = PB[(2 * fc + 1) % 4]
                    kg = ('pb', (2 * fc) % 4); ku = ('pb', (2 * fc + 1) % 4)
                    for dc in range(8):
                        P.op('pe', lambda e, pg=pg, dc=dc, fc=fc: e.matmul(pg[:], lhsT=wgt[wb][:, dc, fc * 128:(fc + 1) * 128], rhs=xn2T[:, dc, tg * 512:(tg + 1) * 512],
                                                                           start=(dc == 0), stop=(dc == 7)),
                             r=[('wgt', wb, dc)] + [('xn2T', tg * 4 + j) for j in range(4)], w=[kg])
                    for dc in range(8):
                        P.op('pe', lambda e, pu=pu, dc=dc, fc=fc: e.matmul(pu[:], lhsT=wgt[wb][:, dc, 512 + fc * 128:512 + (fc + 1) * 128], rhs=xn2T[:, dc, tg * 512:(tg + 1) * 512],
                                                                           start=(dc == 0), stop=(dc == 7)),
                             r=[('wgt', wb, dc)] + [('xn2T', tg * 4 + j) for j in range(4)], w=[ku])
                    b = fc % 2
                    cg = V_BGU + ei * 16 + hf * 4 + fc
                    cu = V_BGU + ei * 16 + 8 + hf * 4 + fc
                    P.op('dve', lambda e, pg=pg, b=b, cg=cg: e.tensor_scalar(out=gc[b][:], in0=pg[:], scalar1=vec[:, cg:cg + 1], scalar2=7.0, op0=ALU.add, op1=ALU.min), r=[kg], w=[('gc', b)])
                    P.op('act', lambda e, b=b: e.activation(out=sg[b][:], in_=gc[b][:], func=AF.Sigmoid, scale=1.702), r=[('gc', b)], w=[('sg', b)])
                    P.op('dve', lambda e, pu=pu, b=b, cu=cu: e.tensor_scalar(out=u1[b][:], in0=pu[:], scalar1=vec[:, cu:cu + 1], scalar2=-7.0, op0=ALU.add, op1=ALU.max), r=[ku], w=[('u1', b)])
                    P.op('dve', lambda e, b=b: e.tensor_scalar(out=u1[b][:], in0=u1[b][:], scalar1=7.0, scalar2=1.0, op0=ALU.min, op1=ALU.add), r=[('u1', b)], w=[('u1', b)])
                    P.op('pool', lambda e, b=b: e.tensor_tensor(out=gc[b][:], in0=gc[b][:], in1=sg[b][:], op=ALU.mult), r=[('gc', b), ('sg', b)], w=[('gc', b)])
                    P.op('pool', lambda e, b=b, fc=fc: e.tensor_tensor(out=actT[ab][:, fc, :], in0=gc[b][:], in1=u1[b][:], op=ALU.mult), r=[('gc', b), ('u1', b)], w=[('actT', ab, fc)])

            def DN(si, tg, ab):
                ei, hf = seq[si]
                wb = si % 2
                for j in range(4):
                    blk = tg * 4 + j
                    for n in range(2):
                        pi = 4 + (j * 2 + n) % 2
                        pb = PB[pi]
                        for fc in range(4):
                            P.op('pe', lambda e, pb=pb, fc=fc, j=j, n=n: e.matmul(pb[:], lhsT=actT[ab][:, fc, j * 128:(j + 1) * 128], rhs=wdt[wb][:, fc, n * 512:(n + 1) * 512],
                                                                                  start=(fc == 0), stop=(fc == 3)),
                                 r=[('actT', ab, fc), ('wdt', wb, fc)], w=[('pb', pi)])
                        P.op('dve', lambda e, pb=pb, blk=blk, n=n: e.scalar_tensor_tensor(out=H[:, blk, n * 512:(n + 1) * 512], in0=pb[:], scalar=G[:, blk, ei:ei + 1],
                                                                                          in1=H[:, blk, n * 512:(n + 1) * 512], op0=ALU.mult, op1=ALU.add),
                             r=[('pb', pi), ('G', blk), ('H', blk)], w=[('H', blk)])

            load_expert(0, 0)
            units = [(si, tg) for si in range(len(seq)) for tg in range(4)]
            for ui in range(len(units) + 1):
                if ui < len(units):
                    si, tg = units[ui]
                    GU(si, tg, ui % 2)
                if ui >= 1:
                    psi, ptg = units[ui - 1]
                    DN(psi, ptg, (ui - 1) % 2)
                if ui < len(units):
                    si, tg = units[ui]
                    if tg == 0 and si + 1 < len(seq):
                        load_expert(*seq[si + 1])
            if DBG:
                h2_d = dscr("h2_d", [2048, 1024], F32)
                for oc in range(4):
                    P.dma('sp', h2_d[oc * 512:(oc + 1) * 512, :].rearrange("(j p) d -> p j d", p=128), H[:, oc * 4:(oc + 1) * 4, :],
                          r=[('H', oc * 4 + j) for j in range(4)], w=[('h2_d', oc)])
                g_d = dscr("g_d", [128, 16, 32], F32)
                P.dma('sp', g_d[:, :, :], G[:], r=[('G', b_) for b_ in range(16)], w=['g_d'])
            P.barrier()
            P.emit()

            ph.close()
            if STOP == 5:
                phH.close()
                return finish_early()
            with ExitStack() as ph2:
                sb2 = lambda name, shape, dt=F32: ph2.enter_context(_sbuf(name, shape, dt))
                wpg = sb2("wpg", [128, 8, 1024], BF16); wpp = sb2("wpp", [128, 2, 1024], BF16)
                bpg = sb2("bpg", [1, 1024]); gpo = sb2("gpo", [128, 1024]); gfi = sb2("gfi", [128, 1024])
                hs = sb2("hs", [128, 1024], BF16); hnT = sb2("hnT", [128, 8, 128], BF16)
                pt_ = sb2("pt_", [128, 256]); ptb = sb2("ptb_", [128, 256], BF16); pT = sb2("pT", [128, 2, 128], BF16)
                gate = sb2("gate", [128, 1024]); plr = sb2("plr", [128, 1024]); ot = [sb2("ot%d" % i, [128, 1024]) for i in range(2)]
                load_w_bf16(wpg, w_plg, 0, 8, 0, 1024, 'wpg')
                load_w_bf16(wpp, w_plp, 0, 2, 0, 1024, 'wpp')
                P.dma('sp', bpg[:], b_plg[:, :], w=['bpg'])
                P.dma('sp', gpo[:], g_post.partition_broadcast(128), w=['gpo'])
                P.dma('sp', gfi[:], g_fin.partition_broadcast(128), w=['gfi'])

                def rstd_blk(src_ap, rk, col):
                    P.op('act', lambda e: e.activation(out=junkE[:], in_=src_ap, func=AF.Square, accum_out=sm['ss'][:, col:col + 1]), r=rk, w=['junkE', ('ss', col)])
                    P.op('dve', lambda e: e.tensor_scalar(out=sm['ms'][:, col:col + 1], in0=sm['ss'][:, col:col + 1], scalar1=1.0 / 1024, scalar2=EPS, op0=ALU.mult, op1=ALU.add),
                         r=[('ss', col)], w=[('ms', col)])
                    P.op('act', lambda e: e.activation(out=sm['sq'][:, col:col + 1], in_=sm['ms'][:, col:col + 1], func=AF.Sqrt), r=[('ms', col)], w=[('sq', col)])
                    P.op('dve', lambda e: e.reciprocal(out=sm['rstd'][:, col:col + 1], in_=sm['sq'][:, col:col + 1]), r=[('sq', col)], w=[('rstd', col)])

                for blk in range(16):
                    Hb = H[:, blk, :]
                    rstd_blk(Hb, [('H', blk)], 1)
                    P.op('act', lambda e, Hb=Hb: e.activation(out=hs[:], in_=Hb, func=AF.Copy, scale=sm['rstd'][:, 1:2]), r=[('H', blk), ('rstd', 1)], w=['hs'])
                    for dc in range(8):
                        P.op('pe', lambda e, dc=dc: e.transpose(PT[:, dc * 128:(dc + 1) * 128], hs[:, dc * 128:(dc + 1) * 128], identb[:]), r=['hs', 'identb'], w=['ptP'])
                    for dc in range(8):
                        P.op('dve', lambda e, dc=dc: e.tensor_scalar(out=hnT[:, dc, :], in0=PT[:, dc * 128:(dc + 1) * 128], scalar1=vec[:, V_GPL + dc:V_GPL + dc + 1], scalar2=None, op0=ALU.mult),
                             r=['ptP'], w=[('hnT', dc)])
                    for n in range(2):
                        pb = PB[n]
                        for dc in range(8):
                            P.op('pe', lambda e, pb=pb, dc=dc, n=n: e.matmul(pb[:], lhsT=hnT[:, dc, :], rhs=wpg[:, dc, n * 512:(n + 1) * 512], start=(dc == 0), stop=False),
                                 r=[('hnT', dc), ('wpg', dc)], w=[('pb', n)])
                        P.op('pe', lambda e, pb=pb, n=n: e.matmul(pb[:], lhsT=ones1[:], rhs=bpg[:, n * 512:(n + 1) * 512], start=False, stop=True), r=['ones1', 'bpg'], w=[('pb', n)])
                        P.op('act', lambda e, pb=pb, n=n: e.activation(out=gate[:, n * 512:(n + 1) * 512], in_=pb[:], func=AF.Sigmoid), r=[('pb', n)], w=['gate'])
                    P.dma('sp', pt_[:], po[blk * 128:(blk + 1) * 128, :], w=['pt_'])
                    P.op('pool', lambda e: e.tensor_copy(out=ptb[:], in_=pt_[:]), r=['pt_'], w=['ptb'])
                    for kc in range(2):
                        P.op('pe', lambda e, kc=kc: e.transpose(PTs[1][:, kc * 128:(kc + 1) * 128], ptb[:, kc * 128:(kc + 1) * 128], identb[:]), r=['ptb', 'identb'], w=['ptP1'])
                    P.op('dve', lambda e: e.tensor_copy(out=pT[:], in_=PTs[1][:, 0:256].rearrange("p (c q) -> p c q", c=2)), r=['ptP1'], w=['pT'])
                    for n in range(2):
                        pb = PB[2 + n]
                        for kc in range(2):
                            P.op('pe', lambda e, pb=pb, kc=kc, n=n: e.matmul(pb[:], lhsT=pT[:, kc, :], rhs=wpp[:, kc, n * 512:(n + 1) * 512], start=(kc == 0), stop=(kc == 1)),
                                 r=['pT', ('wpp', kc)], w=[('pb', 2 + n)])
                        P.op('act', lambda e, pb=pb, n=n: e.copy(out=plr[:, n * 512:(n + 1) * 512], in_=pb[:]), r=[('pb', 2 + n)], w=['plr'])
                    rstd_blk(plr[:], ['plr'], 2)
                    P.op('dve', lambda e: e.scalar_tensor_tensor(out=plr[:], in0=plr[:], scalar=sm['rstd'][:, 2:3], in1=gpo[:], op0=ALU.mult, op1=ALU.mult),
                         r=['plr', ('rstd', 2), 'gpo'], w=['plr'])
                    P.op('pool', lambda e: e.tensor_tensor(out=plr[:], in0=plr[:], in1=gate[:], op=ALU.mult), r=['plr', 'gate'], w=['plr'])
                    P.op('pool', lambda e, Hb=Hb: e.tensor_tensor(out=Hb, in0=Hb, in1=plr[:], op=ALU.add), r=['plr', ('H', blk)], w=[('H', blk)])
                    rstd_blk(Hb, [('H', blk)], 3)
                    o = ot[blk % 2]
                    P.op('dve', lambda e, Hb=Hb, o=o: e.scalar_tensor_tensor(out=o[:], in0=Hb, scalar=sm['rstd'][:, 3:4], in1=gfi[:], op0=ALU.mult, op1=ALU.mult),
                         r=[('H', blk), ('rstd', 3), 'gfi'], w=[('ot', blk % 2)])
                    P.dma('sp', out_d[blk * 128:(blk + 1) * 128, :], o[:], r=[('ot', blk % 2)], w=[('out', blk)])
                P.barrier()
                P.emit()
            phH.close()
    return nc


def _fm(v, n):
    return np.ascontiguousarray(np.asarray(v, np.float32).reshape(n, 128).T)


def kernel(**inp):
    f = lambda k: np.asarray(inp[k], np.float32)
    x = f("x"); p = f("p")[0]
    vecs = np.zeros((128, NV), np.float32)
    vecs[:, V_GMIX:V_GMIX + 8] = _fm(f("norm_mix_g")[0], 8)
    vecs[:, V_GMOE:V_GMOE + 8] = _fm(f("norm_moe_g")[0], 8)
    vecs[:, V_GPL:V_GPL + 8] = _fm(f("norm_pl_g")[0], 8)
    cw = f("conv_w")[0]
    for j in range(4):
        vecs[:, V_CW + j * 8:V_CW + (j + 1) * 8] = _fm(cw[j], 8)
    vecs[:, V_CB:V_CB + 8] = _fm(f("conv_b")[0], 8)
    vecs[:, V_BA:V_BA + 8] = _fm(f("lru_ba")[0], 8)
    vecs[:, V_BX:V_BX + 8] = _fm(f("lru_bx")[0], 8)
    vecs[:, V_LAM:V_LAM + 8] = _fm(f("lru_lambda")[0], 8)
    vecs[:, V_BM:V_BM + 16] = _fm(f("b_merge")[0], 16)
    bgu = f("b_gate_up")[0]
    for e in range(32):
        vecs[:, V_BGU + e * 16:V_BGU + (e + 1) * 16] = _fm(bgu[e], 16)
    ident = np.eye(128, dtype=np.float32)
    ntri = -np.tril(np.ones((128, 128), np.float32))
    tri_mask = np.triu(np.ones((128, 128), np.float32), 1)
    common = {
        "ident": ident, "ntri": ntri, "vecs": vecs,
        "w_in": f("w_in")[0], "lru_wa": f("lru_wa")[0], "lru_wx": f("lru_wx")[0],
        "w_att_out": f("w_att_out")[0], "w_lru_out": f("w_lru_out")[0], "w_merge": f("w_merge")[0], "w_out": f("w_out")[0],
        "w_router": f("w_router")[0], "b_router": f("b_router")[0].reshape(1, 32),
        "w_gate_up": f("w_gate_up")[0], "w_down": f("w_down")[0], "b_down": f("b_down")[0],
        "w_pl_gate": f("w_pl_gate")[0], "b_pl_gate": f("b_pl_gate")[0].reshape(1, 1024), "w_pl_proj": f("w_pl_proj")[0],
        "g_post": f("norm_pl_post_g")[0].reshape(1, 1024), "g_fin": f("norm_final_g").reshape(1, 1024),
    }
    in_maps = []
    own_rows = []
    for c in range(8):
        b, r = c // 4, c % 4
        blks = []
        for i in range(8):
            blks += [8 * i + r, 8 * i + 7 - r]
        rows = np.concatenate([np.arange(bk * 128, (bk + 1) * 128) for bk in blks])
        own_rows.append((b, rows))
        masks = np.zeros((128, 8, 128), np.float32)
        sel = np.zeros((128, 8), np.float32)
        for pp in range(4):
            masks[:, pp, :] = 1.0 if pp < r else (tri_mask if pp == r else 0.0)
            masks[:, 4 + pp, :] = 1.0 if pp < 3 - r else (tri_mask if pp == 3 - r else 0.0)
        sel[:, r] = 1.0
        sel[:, 4 + 3 - r] = 1.0
        m = dict(common)
        m["xb"] = np.ascontiguousarray(x[b])
        m["xo"] = np.ascontiguousarray(x[b][rows])
        m["po"] = np.ascontiguousarray(p[b][rows])
        m["masks"] = masks.reshape(128, 1024)
        m["sel"] = sel
        in_maps.append(m)
    nc = build_program()
    res = run_bass_kernel_spmd(nc, in_maps, core_ids=list(range(8)))
    if DBG:
        LAST['res'] = res.results
        LAST['own_rows'] = own_rows
    out = np.zeros((2, 8192, 1024), np.float32)
    for c in range(8):
        b, rows = own_rows[c]
        out[b, rows] = np.asarray(res.results[c]["out"], np.float32)
    return out
```
